# Optimizing a Trainium2 kernel written in Bass

```python
import math
import jax
import jax.numpy as jnp
from jax import lax
import numpy as np

D_MODEL = 1024
BATCH = 8
SEQ = 4096
DEPTH = 4

GRID_W = 64
CTX_LEN = 256
EPS = 1e-6
NEG_INF = -1e30

NA_HEADS = 8
NA_HEAD_DIM = 64
NA_WIN_ROWS = 8
NA_WIN_COLS = 16
NA_WIDTH = NA_HEADS * NA_HEAD_DIM

ML_HEADS = 4
ML_HEAD_DIM = 128
ML_WIDTH = ML_HEADS * ML_HEAD_DIM
ML_CHUNK = 128
ML_CONV = 3
ML_GATES = 4 * ML_HEADS

EVEN_IN = 3 * NA_WIDTH + 4 * ML_WIDTH + ML_GATES
EVEN_SPLITS = (NA_WIDTH, 2 * NA_WIDTH, 3 * NA_WIDTH,
               3 * NA_WIDTH + 2 * ML_WIDTH, 3 * NA_WIDTH + 3 * ML_WIDTH,
               3 * NA_WIDTH + 4 * ML_WIDTH)
EVEN_OUT = NA_WIDTH + ML_WIDTH

MLA_HEADS = 16
MLA_NOPE = 64
MLA_ROPE = 32
MLA_V = 64
MLA_QK_DIM = MLA_NOPE + MLA_ROPE
MLA_Q_RANK = 256
MLA_KV_RANK = 128
ATTN_BLOCK = 128
ROPE_BASE = 10000.0

FFN_HIDDEN = 2816
FFN_CONV = 3

kernel_name = "hybrid_na_mlstm_mla_dit_block"


def rms_norm(x, g):
    xf = x.astype(jnp.float32)
    y = xf * lax.rsqrt(jnp.mean(xf * xf, axis=-1, keepdims=True) + EPS)
    return (y * g.astype(jnp.float32)).astype(x.dtype)


def modulate(h, shift, scale):
    return h * (1 + scale) + shift


def dwconv_centred(x, w, b):
    k = w.shape[0]
    p = k // 2
    t = x.shape[1]
    xp = jnp.pad(x, ((0, 0), (p, p), (0, 0)))
    y = b + xp[:, 0:t] * w[0]
    for j in range(1, k):
        y = y + xp[:, j:j + t] * w[j]
    return y


def _rotate(x, pos):
    d = x.shape[-1]
    half = d // 2
    inv = ROPE_BASE ** (-jnp.arange(half, dtype=jnp.float32) / half)
    ang = pos.astype(jnp.float32)[:, None] * inv
    ang = ang.reshape((ang.shape[0],) + (1,) * (x.ndim - 3) + (half,))
    cos = jnp.cos(ang).astype(x.dtype)
    sin = jnp.sin(ang).astype(x.dtype)
    x1, x2 = x[..., :half], x[..., half:]
    return jnp.concatenate([x1 * cos - x2 * sin, x1 * sin + x2 * cos], axis=-1)


def rope_2d(x, row, col):
    a = x.shape[-1] // 2
    return jnp.concatenate([_rotate(x[..., :a], row), _rotate(x[..., a:], col)], axis=-1)


def dense_attention(q, k, v):
    s = jnp.einsum('bqhd,bkhd->bhqk', q, k).astype(jnp.float32) * (q.shape[-1] ** -0.5)
    p = jax.nn.softmax(s, axis=-1).astype(v.dtype)
    o = jnp.einsum('bhqk,bkhd->bqhd', p, v)
    return o.reshape(o.shape[0], o.shape[1], -1)


def blocked_attention(q, k, v):
    b, t, h, d = q.shape
    nb = t // ATTN_BLOCK
    qb = jnp.moveaxis(q.reshape(b, nb, ATTN_BLOCK, h, d), 1, 0)
    scale = d ** -0.5

    def one_block(qi):
        s = jnp.einsum('bqhd,bkhd->bhqk', qi, k).astype(jnp.float32) * scale
        p = jax.nn.softmax(s, axis=-1).astype(v.dtype)
        return jnp.einsum('bhqk,bkhd->bqhd', p, v)

    o = lax.map(one_block, qb)
    return jnp.moveaxis(o, 0, 1).reshape(b, t, -1)


def neighbourhood_attention(q, k, v, k_c, v_c, rpb):
    b, t, h, d = q.shape
    rows = t // GRID_W
    kh = min(NA_WIN_ROWS, rows)
    kw = NA_WIN_COLS
    scale = d ** -0.5
    qg = q.reshape(b, rows, GRID_W, h, d)
    kg = k.reshape(b, rows, GRID_W, h, d)
    vg = v.reshape(b, rows, GRID_W, h, d)
    col = jnp.arange(GRID_W)
    cs = jnp.clip(col - kw // 2, 0, GRID_W - kw)
    in_win = (col[None, :] >= cs[:, None]) & (col[None, :] < cs[:, None] + kw)
    dc_idx = jnp.clip(col[None, :] - col[:, None], -(kw - 1), kw - 1) + (NA_WIN_COLS - 1)
    rpb_cols = rpb.astype(jnp.float32)[:, :, dc_idx]

    def row_block(args):
        r, q_row = args
        rs = jnp.clip(r - kh // 2, 0, rows - kh)
        k_band = lax.dynamic_slice_in_dim(kg, rs, kh, axis=1).reshape(b, kh * GRID_W, h, d)
        v_band = lax.dynamic_slice_in_dim(vg, rs, kh, axis=1).reshape(b, kh * GRID_W, h, d)
        dr_idx = rs + jnp.arange(kh) - r + (NA_WIN_ROWS - 1)
        bias = jnp.take(rpb_cols, dr_idx, axis=1)
        bias = jnp.where(in_win[None, None], bias, NEG_INF)
        bias = bias.transpose(0, 2, 1, 3).reshape(h, GRID_W, kh * GRID_W)
        s_lat = jnp.einsum('bqhd,bkhd->bhqk', q_row, k_band).astype(jnp.float32) * scale + bias
        s_ctx = jnp.einsum('bqhd,bkhd->bhqk', q_row, k_c).astype(jnp.float32) * scale
        p = jax.nn.softmax(jnp.concatenate([s_lat, s_ctx], axis=-1), axis=-1).astype(v.dtype)
        nl = kh * GRID_W
        return (jnp.einsum('bhqk,bkhd->bqhd', p[..., :nl], v_band)
                + jnp.einsum('bhqk,bkhd->bqhd', p[..., nl:], v_c))

    o = lax.map(row_block, (jnp.arange(rows), jnp.moveaxis(qg, 1, 0)))
    return jnp.moveaxis(o, 0, 1).reshape(b, t, h * d)


def mlstm_chunked(q, k, v, i_pre, logf, state):
    b, h, t, d = q.shape
    nc = t // ML_CHUNK

    def to_chunks(a):
        return jnp.moveaxis(a.reshape(a.shape[:2] + (nc, ML_CHUNK) + a.shape[3:]), 2, 0)

    xs = tuple(to_chunks(a) for a in (q, k, v, i_pre, logf))
    lower = jnp.tril(jnp.ones((ML_CHUNK, ML_CHUNK), dtype=bool))

    def step(carry, chunk):
        c_st, n_st, m_st = carry
        qc, kc, vc, ic, fc = chunk
        bcum = jnp.cumsum(fc, axis=-1)
        d_in = jnp.where(lower, bcum[..., :, None] - bcum[..., None, :] + ic[..., None, :], -jnp.inf)
        m_inter = bcum + m_st[..., None]
        m_t = jnp.maximum(m_inter, jnp.max(d_in, axis=-1))
        w = jnp.exp(d_in - m_t[..., None])
        a = jnp.exp(m_inter - m_t)
        qk = jnp.einsum('bhtd,bhsd->bhts', qc, kc) * w
        num = (a[..., None] * jnp.einsum('bhvd,bhtd->bhtv', c_st, qc)
               + jnp.einsum('bhts,bhsv->bhtv', qk, vc))
        den = a * jnp.einsum('bhd,bhtd->bht', n_st, qc) + jnp.sum(qk, axis=-1)
        h_out = num / jnp.maximum(jnp.abs(den), jnp.exp(-m_t))[..., None]
        b_last = bcum[..., -1]
        g = b_last[..., None] - bcum + ic
        m_new = jnp.maximum(b_last + m_st, jnp.max(g, axis=-1))
        decay = jnp.exp(b_last + m_st - m_new)
        wk = jnp.exp(g - m_new[..., None])
        c_new = decay[..., None, None] * c_st + jnp.einsum('bhs,bhsv,bhsd->bhvd', wk, vc, kc)
        n_new = decay[..., None] * n_st + jnp.einsum('bhs,bhsd->bhd', wk, kc)
        return (c_new, n_new, m_new), h_out

    state, hs = lax.scan(step, state, xs)
    return jnp.moveaxis(hs, 0, 2).reshape(b, h, t, d), state


def _ml_heads(a):
    b, t, _ = a.shape
    return a.reshape(b, t, ML_HEADS, ML_HEAD_DIM).transpose(0, 2, 1, 3).astype(jnp.float32)


def _rev(a):
    return jnp.flip(a, axis=2)


def _mlstm_inputs(qk, v, g, conv_w, conv_b):
    qk = jax.nn.silu(dwconv_centred(qk, conv_w, conv_b))
    q, k = jnp.split(qk, 2, axis=-1)
    q = _ml_heads(q) * (ML_HEAD_DIM ** -0.5)
    g = g.astype(jnp.float32).transpose(0, 2, 1)
    i_f, f_f, i_b, f_b = jnp.split(g, 4, axis=1)
    return (_ml_heads(q.transpose(0, 2, 1, 3).reshape(q.shape[0], q.shape[2], ML_WIDTH)),
            _ml_heads(k), _ml_heads(v),
            i_f, jax.nn.log_sigmoid(f_f), i_b, jax.nn.log_sigmoid(f_b))


def _mlstm_output(h, o_pre, g):
    b, nh, t, d = h.shape
    o = jax.nn.sigmoid(o_pre.astype(jnp.float32)).reshape(b, t, nh, d).transpose(0, 2, 1, 3)
    h = o * h
    mu = jnp.mean(h, axis=-1, keepdims=True)
    var = jnp.mean(jnp.square(h - mu), axis=-1, keepdims=True)
    h = (h - mu) * lax.rsqrt(var + EPS)
    h = h.transpose(0, 2, 1, 3).reshape(b, t, nh * d) * g.astype(jnp.float32)
    return h.astype(o_pre.dtype)


def _zero_state(b):
    return (jnp.zeros((b, ML_HEADS, ML_HEAD_DIM, ML_HEAD_DIM), jnp.float32),
            jnp.zeros((b, ML_HEADS, ML_HEAD_DIM), jnp.float32),
            jnp.zeros((b, ML_HEADS), jnp.float32))


def even_mixer(hx, hc, w_in, gate_b, conv_w, conv_b, rpb, ml_norm_g, w_out, ctx_out):
    naq_x, nak_x, nav_x, mlqk_x, mlv_x, mlo_x, mlg_x = jnp.split(hx @ w_in, list(EVEN_SPLITS), axis=-1)
    naq_c, nak_c, nav_c, mlqk_c, mlv_c, mlo_c, mlg_c = jnp.split(hc @ w_in, list(EVEN_SPLITS), axis=-1)

    def heads(a):
        return a.reshape(a.shape[0], a.shape[1], NA_HEADS, NA_HEAD_DIM)

    na_x = neighbourhood_attention(heads(naq_x), heads(nak_x), heads(nav_x),
                                   heads(nak_c), heads(nav_c), rpb)

    q_x, k_x, v_x, if_x, lf_x, ib_x, lb_x = _mlstm_inputs(mlqk_x, mlv_x, mlg_x + gate_b, conv_w, conv_b)
    q_c, k_c, v_c, if_c, lf_c, ib_c, lb_c = _mlstm_inputs(mlqk_c, mlv_c, mlg_c + gate_b, conv_w, conv_b)
    zero = _zero_state(hx.shape[0])
    h_cf, st_f = mlstm_chunked(q_c, k_c, v_c, if_c, lf_c, zero)
    h_xf, _ = mlstm_chunked(q_x, k_x, v_x, if_x, lf_x, st_f)
    h_cb, st_b = mlstm_chunked(_rev(q_c), _rev(k_c), _rev(v_c), _rev(ib_c), _rev(lb_c), zero)
    h_xb, _ = mlstm_chunked(_rev(q_x), _rev(k_x), _rev(v_x), _rev(ib_x), _rev(lb_x), st_b)
    ml_x = _mlstm_output(h_xf + _rev(h_xb), mlo_x, ml_norm_g)
    y_x = jnp.concatenate([na_x, ml_x], axis=-1) @ w_out
    if not ctx_out:
        return y_x, None
    na_c = dense_attention(heads(naq_c), heads(nak_c), heads(nav_c))
    ml_c = _mlstm_output(h_cf + _rev(h_cb), mlo_c, ml_norm_g)
    y_c = jnp.concatenate([na_c, ml_c], axis=-1) @ w_out
    return y_x, y_c


def _mla_q(h, w_dq, q_norm_g, w_uq, row, col):
    b, t, _ = h.shape
    q = (rms_norm(h @ w_dq, q_norm_g) @ w_uq).reshape(b, t, MLA_HEADS, MLA_QK_DIM)
    if row is None:
        return q
    return jnp.concatenate([q[..., :MLA_NOPE], rope_2d(q[..., MLA_NOPE:], row, col)], axis=-1)


def _mla_kv(h, w_dkv, kv_norm_g, w_ukv, row, col):
    b, t, _ = h.shape
    ckv = h @ w_dkv
    c_kv, k_pe = ckv[..., :MLA_KV_RANK], ckv[..., MLA_KV_RANK:]
    if row is not None:
        k_pe = rope_2d(k_pe, row, col)
    kv = (rms_norm(c_kv, kv_norm_g) @ w_ukv).reshape(b, t, MLA_HEADS, MLA_NOPE + MLA_V)
    k = jnp.concatenate([kv[..., :MLA_NOPE],
                         jnp.broadcast_to(k_pe[:, :, None, :], (b, t, MLA_HEADS, MLA_ROPE))], axis=-1)
    return k, kv[..., MLA_NOPE:]


def odd_mixer(hx, hc, w_dq, q_norm_g, w_uq, w_dkv, kv_norm_g, w_ukv, w_o, row, col, ctx_out):
    q_x = _mla_q(hx, w_dq, q_norm_g, w_uq, row, col)
    k_x, v_x = _mla_kv(hx, w_dkv, kv_norm_g, w_ukv, row, col)
    k_c, v_c = _mla_kv(hc, w_dkv, kv_norm_g, w_ukv, None, None)
    y_x = blocked_attention(q_x, jnp.concatenate([k_x, k_c], axis=1),
                            jnp.concatenate([v_x, v_c], axis=1)) @ w_o
    if not ctx_out:
        return y_x, None
    q_c = _mla_q(hc, w_dq, q_norm_g, w_uq, None, None)
    return y_x, dense_attention(q_c, k_c, v_c) @ w_o


def conv_ffn(h, w_up, conv_w, conv_b, w_down):
    u = dwconv_centred(h @ w_up, conv_w, conv_b)
    a, g = jnp.split(u, 2, axis=-1)
    return (a * jax.nn.silu(g)) @ w_down


def setup_inputs(seed: int = 0) -> dict:
    key = jax.random.key(seed)
    ks = jax.random.split(key, 32)
    n_even = (DEPTH + 1) // 2
    n_odd = DEPTH // 2
    f32 = jnp.float32

    def nrm(k, shape, scale):
        return jax.random.normal(k, shape, f32) * scale

    gk = jax.random.split(ks[15], 4)
    forget_b = jnp.linspace(3.0, 6.0, ML_HEADS, dtype=f32)
    gate_b = jnp.concatenate([
        nrm(gk[0], (n_even, ML_HEADS), 0.1),
        forget_b + nrm(gk[1], (n_even, ML_HEADS), 0.1),
        nrm(gk[2], (n_even, ML_HEADS), 0.1),
        forget_b + nrm(gk[3], (n_even, ML_HEADS), 0.1)], axis=-1)
    return {
        "x": nrm(ks[0], (BATCH, SEQ, D_MODEL), 1.0),
        "c": nrm(ks[1], (BATCH, D_MODEL), 1.0),
        "ctx": nrm(ks[2], (BATCH, CTX_LEN, D_MODEL), 1.0),
        "c_ctx": nrm(ks[3], (D_MODEL,), 1.0),
        "ada_w": nrm(ks[4], (DEPTH, D_MODEL, 6 * D_MODEL), D_MODEL ** -0.5),
        "ada_b": nrm(ks[5], (DEPTH, 6 * D_MODEL), 0.02),
        "norm_g": 1.0 + nrm(ks[6], (DEPTH, 4, D_MODEL), 0.05),
        "ffn_w_up": nrm(ks[7], (DEPTH, D_MODEL, 2 * FFN_HIDDEN), D_MODEL ** -0.5),
        "ffn_conv_w": nrm(ks[8], (DEPTH, FFN_CONV, 2 * FFN_HIDDEN), FFN_CONV ** -0.5),
        "ffn_conv_b": nrm(ks[9], (DEPTH, 2 * FFN_HIDDEN), 0.02),
        "ffn_w_down": nrm(ks[10], (DEPTH, FFN_HIDDEN, D_MODEL), FFN_HIDDEN ** -0.5),
        "ev_w_in": nrm(ks[11], (n_even, D_MODEL, EVEN_IN), D_MODEL ** -0.5),
        "ev_gate_b": gate_b,
        "ev_conv_w": nrm(ks[12], (n_even, ML_CONV, 2 * ML_WIDTH), ML_CONV ** -0.5),
        "ev_conv_b": nrm(ks[13], (n_even, 2 * ML_WIDTH), 0.02),
        "ev_rpb": nrm(ks[14], (n_even, NA_HEADS, 2 * NA_WIN_ROWS - 1, 2 * NA_WIN_COLS - 1), 0.1),
        "ev_ml_norm_g": 1.0 + nrm(ks[16], (n_even, ML_WIDTH), 0.05),
        "ev_w_out": nrm(ks[17], (n_even, EVEN_OUT, D_MODEL), EVEN_OUT ** -0.5),
        "od_w_dq": nrm(ks[18], (n_odd, D_MODEL, MLA_Q_RANK), D_MODEL ** -0.5),
        "od_q_norm_g": 1.0 + nrm(ks[19], (n_odd, MLA_Q_RANK), 0.05),
        "od_w_uq": nrm(ks[20], (n_odd, MLA_Q_RANK, MLA_HEADS * MLA_QK_DIM), MLA_Q_RANK ** -0.5),
        "od_w_dkv": nrm(ks[21], (n_odd, D_MODEL, MLA_KV_RANK + MLA_ROPE), D_MODEL ** -0.5),
        "od_kv_norm_g": 1.0 + nrm(ks[22], (n_odd, MLA_KV_RANK), 0.05),
        "od_w_ukv": nrm(ks[23], (n_odd, MLA_KV_RANK, MLA_HEADS * (MLA_NOPE + MLA_V)), MLA_KV_RANK ** -0.5),
        "od_w_o": nrm(ks[24], (n_odd, MLA_HEADS * MLA_V, D_MODEL), (MLA_HEADS * MLA_V) ** -0.5),
    }


def reference(x, c, ctx, c_ctx, ada_w, ada_b, norm_g, ffn_w_up, ffn_conv_w, ffn_conv_b, ffn_w_down,
              ev_w_in, ev_gate_b, ev_conv_w, ev_conv_b, ev_rpb, ev_ml_norm_g, ev_w_out,
              od_w_dq, od_q_norm_g, od_w_uq, od_w_dkv, od_kv_norm_g, od_w_ukv, od_w_o):
    t = x.shape[1]
    pos = jnp.arange(t)
    row = pos // GRID_W
    col = pos % GRID_W
    silu_c = jax.nn.silu(c)
    silu_cc = jax.nn.silu(c_ctx)
    xc = ctx
    for l in range(DEPTH):
        ctx_out = l < DEPTH - 1
        mod_x = (silu_c @ ada_w[l] + ada_b[l])[:, None, :]
        mod_c = silu_cc @ ada_w[l] + ada_b[l]
        sh1, sc1, g1, sh2, sc2, g2 = jnp.split(mod_x, 6, axis=-1)
        csh1, csc1, cg1, csh2, csc2, cg2 = jnp.split(mod_c, 6, axis=-1)
        hx = modulate(rms_norm(x, norm_g[l, 0]), sh1, sc1)
        hc = modulate(rms_norm(xc, norm_g[l, 0]), csh1, csc1)
        if l % 2 == 0:
            e = l // 2
            yx, yc = even_mixer(hx, hc, ev_w_in[e], ev_gate_b[e], ev_conv_w[e], ev_conv_b[e],
                                ev_rpb[e], ev_ml_norm_g[e], ev_w_out[e], ctx_out)
        else:
            o = l // 2
            yx, yc = odd_mixer(hx, hc, od_w_dq[o], od_q_norm_g[o], od_w_uq[o], od_w_dkv[o],
                               od_kv_norm_g[o], od_w_ukv[o], od_w_o[o], row, col, ctx_out)
        x = x + g1 * rms_norm(yx, norm_g[l, 1])
        hx = modulate(rms_norm(x, norm_g[l, 2]), sh2, sc2)
        x = x + g2 * rms_norm(conv_ffn(hx, ffn_w_up[l], ffn_conv_w[l], ffn_conv_b[l], ffn_w_down[l]),
                              norm_g[l, 3])
        if ctx_out:
            xc = xc + cg1 * rms_norm(yc, norm_g[l, 1])
            hc = modulate(rms_norm(xc, norm_g[l, 2]), csh2, csc2)
            xc = xc + cg2 * rms_norm(conv_ffn(hc, ffn_w_up[l], ffn_conv_w[l], ffn_conv_b[l], ffn_w_down[l]),
                                     norm_g[l, 3])
    return x
```

```python
import numpy as np
from contextlib import ExitStack, contextmanager
import concourse.bass as bass
import concourse.mybir as mybir
from concourse.bass_utils import run_bass_kernel_spmd

F32 = mybir.dt.float32
BF16 = mybir.dt.bfloat16
AF = mybir.ActivationFunctionType
ALU = mybir.AluOpType
AX = mybir.AxisListType

T = 4096
TC = 256
TT = T + TC
D = 1024
DEPTH = 4
GRID = 64
EPS = 1e-6
FH = 2816
NDMA = 40


class Tl:
    def __init__(self, h, name):
        self.h = h
        self.name = name

    def __getitem__(self, k):
        return self.h[k]


class Sched:
    def __init__(self, nc, es):
        self.nc = nc
        self.engs = {'pe': nc.tensor, 'dve': nc.vector, 'act': nc.scalar, 'pool': nc.gpsimd, 'sp': nc.sync}
        self.sems = {e: es.enter_context(nc.semaphore("s_" + e)) for e in ('pe', 'dve', 'act', 'pool')}
        self.cnt = {e: 0 for e in self.sems}
        self.known = {e: {} for e in self.engs}
        self.dsems = [es.enter_context(nc.semaphore("d%d" % i)) for i in range(NDMA)]
        self.dval = [0] * NDMA
        self.dnext = 0
        self.dnext_sw = NDMA - 12
        self.lastw = {}
        self.readers = {}
        self.uid = 0
        self.nins = 0

    def _wait(self, e, ev):
        clk, val, snap = ev
        if clk == e and e == 'pe':
            return
        kn = self.known[e]
        if kn.get(clk, 0) >= val:
            return
        sem = self.sems[clk] if isinstance(clk, str) else self.dsems[clk]
        self.engs[e].wait_ge(sem, val)
        self.nins += 1
        new = dict(kn)
        new[clk] = val
        for c_, v_ in snap.items():
            if new.get(c_, 0) < v_:
                new[c_] = v_
        self.known[e] = new

    def _deps(self, e, reads, writes):
        for k in reads:
            ev = self.lastw.get(k)
            if ev is not None:
                self._wait(e, ev)
        for k in writes:
            ev = self.lastw.get(k)
            if ev is not None:
                self._wait(e, ev)
            rd = self.readers.get(k)
            if rd:
                for clk, (val, snap) in rd.items():
                    self._wait(e, (clk, val, snap))

    def _commit(self, ev, reads, writes):
        clk, val, snap = ev
        for k in reads:
            self.readers.setdefault(k, {})[clk] = (val, snap)
        for k in writes:
            self.lastw[k] = ev
            self.readers[k] = {}

    def op(self, e, fns, reads=(), writes=()):
        reads = [getattr(k, 'key', k) for k in reads]
        writes = [getattr(k, 'key', k) for k in writes]
        self._deps(e, reads, writes)
        if callable(fns):
            fns = [fns]
        eng = self.engs[e]
        for f in fns[:-1]:
            f(eng)
        ins = fns[-1](eng)
        self.nins += len(fns)
        self.cnt[e] += 1
        ins.then_inc(self.sems[e], 1)
        self._commit((e, self.cnt[e], self.known[e]), reads, writes)

    def dma(self, q, out, in_, reads=(), writes=(), **kw):
        if q == 'pool':
            i = self.dnext_sw
            self.dnext_sw = NDMA - 12 + (i + 1 - (NDMA - 12)) % 12
        else:
            i = self.dnext
            self.dnext = (i + 1) % (NDMA - 12)
        if self.dval[i] > 0:
            self._wait(q, (i, self.dval[i], {}))
        self._deps(q, reads, writes)
        self.engs[q].dma_start(out=out, in_=in_, **kw).then_inc(self.dsems[i], 16)
        self.nins += 1
        self.dval[i] += 16
        self._commit((i, self.dval[i], self.known[q]), reads, writes)

    def barrier(self):
        for e in self.engs:
            for c_ in self.cnt:
                if self.cnt[c_] > 0:
                    self._wait(e, (c_, self.cnt[c_], {}))
            for i in range(NDMA):
                if self.dval[i] > 0:
                    self._wait(e, (i, self.dval[i], {}))
        self.lastw = {}
        self.readers = {}


class Phase:
    def __init__(self, K):
        self.K = K
        self.es = ExitStack()

    def sb(self, name, shape, dt=F32):
        self.K.S.uid += 1
        h = self.es.enter_context(self.K.nc.sbuf_tensor("%s_%d" % (name, self.K.S.uid), list(shape), dt))
        return Tl(h, name)

    def ps(self, name, shape, dt=F32):
        self.K.S.uid += 1
        h = self.es.enter_context(self.K.nc.psum_tensor("%s_%d" % (name, self.K.S.uid), list(shape), dt))
        return Tl(h, name)


class View:
    def __init__(self, ap, key=None, name="view"):
        self.ap = ap
        self.name = name
        self.key = key if key is not None else self

    def __getitem__(self, k):
        return self.ap[k]


class Rot:
    def __init__(self, tiles):
        self.tiles = tiles
        self.i = 0

    def next(self):
        t = self.tiles[self.i % len(self.tiles)]
        self.i += 1
        return t


class K:
    def __init__(self, dbg=(), nlayers=DEPTH, upto=None):
        self.dbg = set(dbg)
        self.nlayers = nlayers
        self.upto = upto
        self.nc = bass.Bass("TRN2", target_bir_lowering=False)
        self.es = ExitStack()

    def din(self, name, shape):
        return self.nc.dram_tensor(name, list(shape), F32, kind="ExternalInput").ap()

    def dscr(self, name, shape, dt=BF16):
        kind = "ExternalOutput" if name in self.dbg else "Internal"
        return self.nc.dram_tensor(name, list(shape), dt, kind=kind).ap()

    @contextmanager
    def phase(self):
        ph = Phase(self)
        with ph.es:
            yield ph
            self.S.barrier()

    def build(self):
        nc = self.nc
        with self.es:
            self.S = Sched(nc, self.es)
            self.declare()
            self.consts()
            self.cast_weights()
            self.ada_phase()
            if getattr(self, 'plan', None) is not None:
                self.plan(self)
                self.S.barrier()
                return nc
            x_in, xc_in = self.x, self.ctx
            self.norm_only(0, x_in, xc_in)
            for l in range(self.nlayers):
                last = (l == DEPTH - 1)
                if l % 2 == 0:
                    self.even_mixer(l)
                else:
                    self.odd_mixer(l)
                if self.upto == ('mixer', l):
                    break
                self.out_phase(l, 'mix', x_in, xc_in, self.X, self.XC, ctx_out=not last)
                x_in, xc_in = self.X, self.XC
                if self.upto == ('mixout', l):
                    break
                self.ffn1(l, ctx_out=not last)
                if self.upto == ('ffn1', l):
                    break
                self.out_phase(l, 'ffn', x_in, xc_in, (self.out if last else self.X), self.XC, ctx_out=not last)
            self.S.barrier()
        return nc

    def declare(self):
        din = self.din
        self.x = din("x", [T, D])
        self.c = din("c", [1, D])
        self.ctx = din("ctx", [TC, D])
        self.c_ctx = din("c_ctx", [1, D])
        self.ada_w = din("ada_w", [DEPTH, D, 6 * D])
        self.ada_b = din("ada_b", [DEPTH, 6 * D])
        self.norm_g = din("norm_g", [DEPTH, 4 * D])
        self.ffn_w_up = din("ffn_w_up", [DEPTH, D, 2 * FH])
        self.ffn_conv_w = din("ffn_conv_w", [DEPTH, 3, 2 * FH])
        self.ffn_conv_b = din("ffn_conv_b", [DEPTH, 2 * FH])
        self.ffn_w_down = din("ffn_w_down", [DEPTH, FH, D])
        self.ev_w_in = din("ev_w_in", [2, D, 3600])
        self.ev_gate_b = din("ev_gate_b", [2, 16])
        self.ev_conv_w = din("ev_conv_w", [2, 3, 1024])
        self.ev_conv_b = din("ev_conv_b", [2, 1024])
        self.ev_rpb = din("ev_rpb", [2, 120, 31])
        self.ev_ml_norm_g = din("ev_ml_norm_g", [2, 512])
        self.ev_w_out = din("ev_w_out", [2, D, D])
        self.od_w_dq = din("od_w_dq", [2, D, 256])
        self.od_q_norm_g = din("od_q_norm_g", [2, 256])
        self.od_w_uq = din("od_w_uq", [2, 256, 1536])
        self.od_w_dkv = din("od_w_dkv", [2, D, 160])
        self.od_kv_norm_g = din("od_kv_norm_g", [2, 128])
        self.od_w_ukv = din("od_w_ukv", [2, 128, 2048])
        self.od_w_o = din("od_w_o", [2, D, D])
        self.k_ident = din("k_ident", [128, 128])
        self.k_tri = din("k_tri", [4, 128, 128])
        self.k_pos = din("k_pos", [T, 16])
        self.k_inv = din("k_inv", [1, 16])
        self.k_win = din("k_win", [64, 64])
        self.out = self.nc.dram_tensor("out", [T, D], F32, kind="ExternalOutput").ap()
        ds = self.dscr
        self.X = ds("X", [T, D], F32)
        self.wb = {}
        for nm, shp in (("ffn_w_up", [DEPTH, D, 2 * FH]), ("ffn_w_down", [DEPTH, FH, D]), ("ev_w_in", [2, D, 3600]),
                        ("ev_w_out", [2, D, D]), ("od_w_dq", [2, D, 256]), ("od_w_uq", [2, 256, 1536]),
                        ("od_w_dkv", [2, D, 160]), ("od_w_ukv", [2, 128, 2048]), ("od_w_o", [2, D, D])):
            self.wb[nm] = (ds("wb_" + nm, shp), shp)
        self.XC = ds("XC", [TC, D], F32)
        self.VEC = ds("VEC", [DEPTH, 2, 6 * D], F32)
        self.HTx = ds("HTx", [D, T + 2])
        self.HTc = ds("HTc", [D, TC + 2])
        self.ACTT = ds("ACTT", [FH, TT])
        self.OMIX = ds("OMIX", [TT, D])
        self.QTN = ds("QTN", [512, TT])
        self.KTN = ds("KTN", [512, TT])
        self.VN = ds("VN", [TT, 512])
        self.QKTM = ds("QKTM", [1024, TT])
        self.VML = ds("VML", [TT, 512])
        self.OML = ds("OML", [TT, 512])
        self.GML = ds("GML", [TT, 16], F32)
        self.HF = ds("HF", [TT, 512], F32)
        self.HB = ds("HB", [TT, 512], F32)
        self.RB = ds("RB", [120, 8192], F32)
        self.QT = ds("QT", [16, 96, TT])
        self.KT = ds("KT", [16, 96, TT])
        self.VM = ds("VM", [TT, D])

    def consts(self):
        S = self.S
        ph = Phase(self)
        self.cph = ph
        self.es.enter_context(ph.es)
        self.ident = ph.sb("ident", [128, 128], BF16)
        S.dma('pool', self.ident[:], self.k_ident, writes=[self.ident])
        self.zcol = ph.sb("zcol", [128, 8], BF16)
        S.op('pool', lambda e: e.memset(self.zcol[:], 0.0), writes=[self.zcol])
        for HT, n in ((self.HTx, T), (self.HTc, TC)):
            v = HT.rearrange("(k p) t -> p k t", p=128)
            S.dma('sp', v[:, :, 0:1], self.zcol[:].rearrange("p (k o) -> p k o", o=1), reads=[self.zcol],
                  writes=["HTpad"], allow_slow_non_contiguous=True)
            S.dma('sp', v[:, :, n + 1:n + 2], self.zcol[:].rearrange("p (k o) -> p k o", o=1), reads=[self.zcol],
                  writes=["HTpad"], allow_slow_non_contiguous=True)
        import math
        pos = ph.sb("pos", [128, 32, 16], F32)
        inv = ph.sb("inv", [128, 16], F32)
        ang = ph.sb("ang", [128, 32, 16], F32)
        self.COS = ph.sb("COS", [128, 32, 16], F32)
        self.SIN = ph.sb("SIN", [128, 32, 16], F32)
        S.dma('sp', pos[:], self.k_pos.rearrange("(n p) j -> p n j", p=128), writes=[pos], allow_slow_non_contiguous=True)
        S.dma('sp', inv[:], self.k_inv.partition_broadcast(128), writes=[inv])
        S.op('dve', lambda e: e.tensor_tensor(out=ang[:], in0=pos[:], in1=inv[:].unsqueeze(1).broadcast_to([128, 32, 16]),
                                              op=ALU.mult), reads=[pos, inv], writes=[ang])
        kacc = ph.sb("kacc", [128, 32, 16], F32)
        for dst, shift in ((self.SIN, 0.5), (self.COS, 0.75)):
            S.op('dve', lambda e: e.tensor_scalar(out=pos[:], in0=ang[:], scalar1=1.0 / (2 * math.pi), scalar2=shift,
                                                  op0=ALU.mult, op1=ALU.add), reads=[ang], writes=[pos])
            S.op('dve', lambda e: e.memset(kacc[:], 0.5), writes=[kacc])
            for m in range(1, 13):
                S.op('dve', lambda e, m=m: e.scalar_tensor_tensor(out=kacc[:], in0=pos[:], scalar=float(m), in1=kacc[:],
                                                                  op0=ALU.is_ge, op1=ALU.add), reads=[pos, kacc], writes=[kacc])
            S.op('dve', lambda e: e.tensor_tensor(out=pos[:], in0=pos[:], in1=kacc[:], op=ALU.subtract),
                 reads=[pos, kacc], writes=[pos])
            S.op('act', lambda e: e.activation(out=dst[:], in_=pos[:], func=AF.Sin, scale=2 * math.pi), reads=[pos], writes=[dst])
        S.barrier()

    def cast_weights(self):
        S = self.S
        for nm, (dst, shp) in self.wb.items():
            src = getattr(self, nm)
            for i in range(shp[0]):
                if nm.startswith("ev_") and 2 * i >= self.nlayers:
                    continue
                if nm.startswith("od_") and 2 * i + 1 >= self.nlayers:
                    continue
                if nm.startswith("ffn_") and i >= self.nlayers:
                    continue
                for r0 in range(0, shp[1], 256):
                    r1 = min(shp[1], r0 + 256)
                    S.dma('pool', dst[i, r0:r1, :], src[i, r0:r1, :], writes=["wb"])
        S.barrier()

    def ada_phase(self):
        S = self.S
        with self.phase() as ph:
            cT = ph.sb("cT", [128, 8, 2], F32)
            cs = ph.sb("cs", [128, 8, 2], BF16)
            S.dma('sp', cT[:, :, 0], self.c.rearrange("o (k p) -> p (o k)", p=128), writes=[cT],
                  allow_slow_non_contiguous=True)
            S.dma('sp', cT[:, :, 1], self.c_ctx.rearrange("o (k p) -> p (o k)", p=128), writes=[cT],
                  allow_slow_non_contiguous=True)
            S.op('act', lambda e: e.activation(out=cs[:], in_=cT[:], func=AF.Silu), reads=[cT], writes=[cs])
            wts = Rot([ph.sb("adaw%d" % i, [128, 8, 512], BF16) for i in range(3)])
            pss = Rot([ph.ps("adaps%d" % i, [2, 512]) for i in range(2)])
            MOD = ph.sb("MOD", [2, 6 * D], F32)
            ADB = ph.sb("ADB", [2, 6 * D], F32)
            NG = ph.sb("NG", [2, 4 * D], F32)
            VEC = ph.sb("VECs", [2, 6 * D], F32)
            for l in range(self.nlayers):
                S.dma('sp', ADB[:], self.ada_b[l:l + 1, :].partition_broadcast(2), writes=[ADB])
                S.dma('sp', NG[:], self.norm_g[l:l + 1, :].partition_broadcast(2), writes=[NG])
                for nb in range(12):
                    wt = wts.next()
                    S.dma('pool', wt[:], self.ada_w[l][:, nb * 512:(nb + 1) * 512].rearrange("(k p) n -> p k n", p=128),
                          writes=[wt])
                    pt = pss.next()
                    S.op('pe', [(lambda e, k=k: e.matmul(pt[:], lhsT=cs[:, k, :], rhs=wt[:, k, :],
                                                         start=(k == 0), stop=(k == 7))) for k in range(8)],
                         reads=[cs, wt], writes=[pt])
                    S.op('dve', lambda e: e.tensor_tensor(out=MOD[:, nb * 512:(nb + 1) * 512], in0=pt[:],
                                                          in1=ADB[:, nb * 512:(nb + 1) * 512], op=ALU.add),
                         reads=[pt, ADB], writes=[MOD])

                def sl(t, i):
                    return t[:, i * D:(i + 1) * D]
                S.op('dve', lambda e: e.scalar_tensor_tensor(out=sl(VEC, 0), in0=sl(MOD, 1), scalar=1.0, in1=sl(NG, 0),
                                                             op0=ALU.add, op1=ALU.mult), reads=[MOD, NG], writes=[VEC])
                S.op('dve', lambda e: e.tensor_copy(out=sl(VEC, 1), in_=sl(MOD, 0)), reads=[MOD], writes=[VEC])
                S.op('dve', lambda e: e.tensor_tensor(out=sl(VEC, 2), in0=sl(MOD, 2), in1=sl(NG, 1), op=ALU.mult),
                     reads=[MOD, NG], writes=[VEC])
                S.op('dve', lambda e: e.scalar_tensor_tensor(out=sl(VEC, 3), in0=sl(MOD, 4), scalar=1.0, in1=sl(NG, 2),
                                                             op0=ALU.add, op1=ALU.mult), reads=[MOD, NG], writes=[VEC])
                S.op('dve', lambda e: e.tensor_copy(out=sl(VEC, 4), in_=sl(MOD, 3)), reads=[MOD], writes=[VEC])
                S.op('dve', lambda e: e.tensor_tensor(out=sl(VEC, 5), in0=sl(MOD, 5), in1=sl(NG, 3), op=ALU.mult),
                     reads=[MOD, NG], writes=[VEC])
                S.dma('sp', self.VEC[l], VEC[:], reads=[VEC], writes=["VEC"])

    def load_vec(self, ph, l, which, idx, name):
        t = ph.sb(name, [128, D], F32)
        self.S.dma('sp', t[:], self.VEC[l, which:which + 1, idx * D:(idx + 1) * D].partition_broadcast(128),
                   reads=["VEC"], writes=[t])
        return t

    def mk_norm_bufs(self, ph, n=2):
        b = {}
        b['junk'] = Rot([ph.sb("junk%d" % i, [128, D], F32) for i in range(n)])
        b['ss'] = Rot([ph.sb("ss%d" % i, [128, 2], F32) for i in range(2 * n)])
        b['tmp'] = Rot([ph.sb("tmp%d" % i, [128, D], F32) for i in range(n)])
        b['hb'] = Rot([ph.sb("hb%d" % i, [128, D], BF16) for i in range(n)])
        b['tp'] = Rot([ph.ps("tp%d" % i, [128, 8, 128], BF16) for i in range(2)])
        b['hT'] = Rot([ph.sb("hT%d" % i, [128, 8, 512], BF16) for i in range(2)])
        return b

    def rstd_of(self, src, srck, b, n):
        S = self.S
        junk = b['junk'].next()
        ss = b['ss'].next()
        S.op('act', lambda e: e.activation(out=junk[:, 0:n], in_=src, func=AF.Square, accum_out=ss[:, 0:1]),
             reads=[srck], writes=[junk, ss])
        S.op('dve', lambda e: e.tensor_scalar(out=ss[:, 1:2], in0=ss[:, 0:1], scalar1=1.0 / n, scalar2=EPS,
                                              op0=ALU.mult, op1=ALU.add), reads=[ss], writes=[ss])
        S.op('act', lambda e: e.activation(out=ss[:, 1:2], in_=ss[:, 1:2], func=AF.Sqrt), reads=[ss], writes=[ss])
        S.op('dve', lambda e: e.reciprocal(out=ss[:, 1:2], in_=ss[:, 1:2]), reads=[ss], writes=[ss])
        return ss

    def norm_to_hT(self, xt, A, SH, b, hT, col):
        S = self.S
        ss = self.rstd_of(xt[:], xt, b, D)
        tmp = b['tmp'].next()
        hb = b['hb'].next()
        S.op('dve', lambda e: e.scalar_tensor_tensor(out=tmp[:], in0=xt[:], scalar=ss[:, 1:2], in1=A[:],
                                                     op0=ALU.mult, op1=ALU.mult), reads=[xt, ss, A], writes=[tmp])
        S.op('pool', lambda e: e.tensor_tensor(out=hb[:], in0=tmp[:], in1=SH[:], op=ALU.add),
             reads=[tmp, SH], writes=[hb])
        tp = b['tp'].next()
        S.op('pe', [(lambda e, k=k: e.transpose(out=tp[:, k, :], in_=hb[:, k * 128:(k + 1) * 128],
                                                identity=self.ident[:])) for k in range(8)],
             reads=[hb, self.ident], writes=[tp])
        S.op('act', lambda e: e.activation(out=hT[:, :, col:col + 128], in_=tp[:], func=AF.Copy),
             reads=[tp], writes=[hT])

    def store_hT(self, hT, HT, tok0, ncols):
        v = HT.rearrange("(k p) t -> p k t", p=128)
        self.S.dma('sp', v[:, :, 1 + tok0:1 + tok0 + ncols], hT[:, :, 0:ncols], reads=[hT], writes=["HT"])

    def norm_only(self, l, x_in, xc_in):
        S = self.S
        with self.phase() as ph:
            b = self.mk_norm_bufs(ph)
            xts = Rot([ph.sb("xt%d" % i, [128, D], F32) for i in range(3)])
            for which, src, n, HT in ((0, x_in, T, self.HTx), (1, xc_in, TC, self.HTc)):
                A = self.load_vec(ph, l, which, 0, "A")
                SH = self.load_vec(ph, l, which, 1, "SH")
                for tb in range(0, n, 512):
                    nb = min(512, n - tb)
                    hT = b['hT'].next()
                    for j in range(nb // 128):
                        xt = xts.next()
                        S.dma('sp', xt[:], src[tb + j * 128: tb + (j + 1) * 128, :], reads=["X"], writes=[xt])
                        self.norm_to_hT(xt, A, SH, b, hT, j * 128)
                    self.store_hT(hT, HT, tb, nb)

    def out_phase(self, l, kind, x_in, xc_in, x_out, xc_out, ctx_out):
        S = self.S
        with self.phase() as ph:
            if kind == 'mix':
                KC = 8
                wsrc = (self.wb["ev_w_out"][0] if l % 2 == 0 else self.wb["od_w_o"][0])[l // 2]
                gi, nxt = 2, (l, 3, 4)
            else:
                KC = 22
                wsrc = self.wb["ffn_w_down"][0][l]
                gi, nxt = 5, ((l + 1, 0, 1) if l + 1 < DEPTH else None)
            W = ph.sb("W", [128, KC, D], BF16)
            wv = wsrc.rearrange("(k p) n -> p k n", p=128)
            for k0 in range(0, KC, 4):
                k1 = min(KC, k0 + 4)
                S.dma('sp', W[:, k0:k1, :], wv[:, k0:k1, :], reads=["wb"], writes=[W])
            b = self.mk_norm_bufs(ph)
            xts = Rot([ph.sb("xt%d" % i, [128, D], F32) for i in range(2)])
            xns = Rot([ph.sb("xn%d" % i, [128, D], F32) for i in range(2)])
            ys = Rot([ph.ps("y%d" % i, [128, D]) for i in range(2)])
            if kind == 'mix':
                ains = Rot([ph.sb("ain%d" % i, [128, D], BF16) for i in range(2)])
                aTs = Rot([ph.sb("aT%d" % i, [128, 8, 128], BF16) for i in range(2)])
            else:
                aTs = Rot([ph.sb("aT%d" % i, [128, 22, 512], BF16) for i in range(2)])
            segs = [(0, x_in, x_out, T, 0, self.HTx)]
            if ctx_out:
                segs.append((1, xc_in, xc_out, TC, T, self.HTc))
            for which, xi, xo, n, off, HT in segs:
                G = self.load_vec(ph, l, which, gi, "G")
                if nxt is not None:
                    A = self.load_vec(ph, nxt[0], which, nxt[1], "A")
                    SH = self.load_vec(ph, nxt[0], which, nxt[2], "SH")
                for tb in range(0, n, 512):
                    nb = min(512, n - tb)
                    hT = b['hT'].next() if nxt is not None else None
                    if kind == 'ffn':
                        aT = aTs.next()
                        S.dma('sp', aT[:, :, 0:nb],
                              self.ACTT.rearrange("(k p) t -> p k t", p=128)[:, :, off + tb: off + tb + nb],
                              reads=["ACTT"], writes=[aT])
                    for j in range(nb // 128):
                        t0 = tb + j * 128
                        xt = xts.next()
                        S.dma('sp', xt[:], xi[t0:t0 + 128, :], reads=["X"], writes=[xt])
                        if kind == 'mix':
                            ain = ains.next()
                            S.dma('sp', ain[:], self.OMIX[off + t0: off + t0 + 128, :], reads=["OMIX"], writes=[ain])
                            tp = b['tp'].next()
                            S.op('pe', [(lambda e, k=k: e.transpose(out=tp[:, k, :], in_=ain[:, k * 128:(k + 1) * 128],
                                                                    identity=self.ident[:])) for k in range(8)],
                                 reads=[ain, self.ident], writes=[tp])
                            aT = aTs.next()
                            S.op('act', lambda e: e.activation(out=aT[:], in_=tp[:], func=AF.Copy),
                                 reads=[tp], writes=[aT])
                            c0 = 0
                        else:
                            c0 = j * 128
                        y = ys.next()
                        mms = []
                        for hf in range(2):
                            for k in range(KC):
                                mms.append(lambda e, k=k, hf=hf: e.matmul(
                                    y[:, hf * 512:(hf + 1) * 512], lhsT=aT[:, k, c0:c0 + 128],
                                    rhs=W[:, k, hf * 512:(hf + 1) * 512], start=(k == 0), stop=(k == KC - 1)))
                        S.op('pe', mms, reads=[aT, W], writes=[y])
                        ss = self.rstd_of(y[:], y, b, D)
                        tmp = b['tmp'].next()
                        xn = xns.next()
                        S.op('dve', lambda e: e.scalar_tensor_tensor(out=tmp[:], in0=y[:], scalar=ss[:, 1:2], in1=G[:],
                                                                     op0=ALU.mult, op1=ALU.mult),
                             reads=[y, ss, G], writes=[tmp])
                        S.op('pool', lambda e: e.tensor_tensor(out=xn[:], in0=tmp[:], in1=xt[:], op=ALU.add),
                             reads=[tmp, xt], writes=[xn])
                        S.dma('sp', xo[t0:t0 + 128, :], xn[:], reads=[xn], writes=["X"])
                        if nxt is not None:
                            self.norm_to_hT(xn, A, SH, b, hT, j * 128)
                    if nxt is not None:
                        self.store_hT(hT, HT, tb, nb)

    def conv3(self, eng, acc, ub, cw, cb, j, n=512):
        S = self.S
        S.op(eng, lambda e: e.tensor_scalar(out=acc[:, 0:n], in0=ub[:, 1:n + 1], scalar1=cw[:, 1, j:j + 1],
                                            scalar2=cb[:, j:j + 1], op0=ALU.mult, op1=ALU.add),
             reads=[ub, cw, cb], writes=[acc])
        S.op(eng, lambda e: e.scalar_tensor_tensor(out=acc[:, 0:n], in0=ub[:, 0:n], scalar=cw[:, 0, j:j + 1],
                                                   in1=acc[:, 0:n], op0=ALU.mult, op1=ALU.add),
             reads=[ub, cw, acc], writes=[acc])
        S.op(eng, lambda e: e.scalar_tensor_tensor(out=acc[:, 0:n], in0=ub[:, 2:n + 2], scalar=cw[:, 2, j:j + 1],
                                                   in1=acc[:, 0:n], op0=ALU.mult, op1=ALU.add),
             reads=[ub, cw, acc], writes=[acc])

    def mm_halo(self, pm, phl, W, hT, col_lo, col_n, n):
        mms = []
        for k in range(8):
            mms.append(lambda e, k=k: e.matmul(pm[:, 0:n], lhsT=W[:, k, col_lo:col_lo + col_n], rhs=hT[:, k, 1:n + 1],
                                               start=(k == 0), stop=(k == 7)))
        if phl is not None:
            for k in range(8):
                mms.append(lambda e, k=k: e.matmul(phl[:, 0:2], lhsT=W[:, k, col_lo:col_lo + col_n],
                                                   rhs=hT[:, k, 0:n + 2:n + 1], start=(k == 0), stop=(k == 7)))
        return mms

    def ffn1(self, l, ctx_out):
        S = self.S
        NJ = FH // 128
        with self.phase() as ph:
            W = ph.sb("Wup", [128, 8, 2 * FH], BF16)
            wv = self.wb["ffn_w_up"][0][l].rearrange("(k p) n -> p k n", p=128)
            for k in range(8):
                S.dma('sp', W[:, k, :], wv[:, k, :], reads=["wb"], writes=[W])
            cw = ph.sb("cw", [128, 3, 2 * NJ], F32)
            cb = ph.sb("cb", [128, 2 * NJ], F32)
            S.dma('sp', cw[:], self.ffn_conv_w[l].rearrange("a (j p) -> p a j", p=128), writes=[cw],
                  allow_slow_non_contiguous=True)
            S.dma('sp', cb[:], self.ffn_conv_b[l:l + 1, :].rearrange("o (j p) -> p (o j)", p=128), writes=[cb],
                  allow_slow_non_contiguous=True)
            hTs = Rot([ph.sb("hTi%d" % i, [128, 8, 514], BF16) for i in range(2)])
            pms = Rot([ph.ps("pm%d" % i, [128, 512]) for i in range(6)])
            hbank = ph.ps("hbank", [128, 512])
            phs = Rot([View(hbank[:, 8 * i:8 * i + 8], key=hbank) for i in range(8)])
            ubs = Rot([ph.sb("ub%d" % i, [128, 514], F32) for i in range(4)])
            accs = Rot([ph.sb("acc%d" % i, [128, 512], F32) for i in range(4)])
            sgs = Rot([ph.sb("sg%d" % i, [128, 512], F32) for i in range(2)])
            outs = Rot([ph.sb("ao%d" % i, [128, NJ, 512], BF16) for i in range(2)])
            segs = [(self.HTx, T, 0)]
            if ctx_out:
                segs.append((self.HTc, TC, T))
            for HT, n, off in segs:
                hv = HT.rearrange("(k p) t -> p k t", p=128)
                for tb in range(0, n, 512):
                    nb = min(512, n - tb)
                    hT = hTs.next()
                    S.dma('sp', hT[:, :, 0:nb + 2], hv[:, :, tb:tb + nb + 2], reads=["HT", "HTpad"], writes=[hT])
                    ao = outs.next()
                    for j in range(NJ):
                        accl = []
                        for half in range(2):
                            jj = half * NJ + j
                            pm = pms.next()
                            phl = phs.next()
                            S.op('pe', self.mm_halo(pm, phl, W, hT, jj * 128, 128, nb), reads=[W, hT], writes=[pm, phl])
                            ub = ubs.next()
                            S.op('act', lambda e: e.activation(out=ub[:, 1:nb + 1], in_=pm[:, 0:nb], func=AF.Copy),
                                 reads=[pm], writes=[ub])
                            S.op('act', lambda e: e.activation(out=ub[:, 0:nb + 2:nb + 1], in_=phl[:, 0:2], func=AF.Copy),
                                 reads=[phl, ub], writes=[ub])
                            acc = accs.next()
                            self.conv3('dve', acc, ub, cw, cb, jj, nb)
                            accl.append(acc)
                        sg = sgs.next()
                        S.op('act', lambda e: e.activation(out=sg[:, 0:nb], in_=accl[1][:, 0:nb], func=AF.Silu),
                             reads=[accl[1]], writes=[sg])
                        S.op('dve', lambda e: e.tensor_tensor(out=ao[:, j, 0:nb], in0=sg[:, 0:nb], in1=accl[0][:, 0:nb],
                                                              op=ALU.mult), reads=[sg, accl[0]], writes=[ao])
                    S.dma('sp', self.ACTT.rearrange("(k p) t -> p k t", p=128)[:, :, off + tb: off + tb + nb],
                          ao[:, :, 0:nb], reads=[ao], writes=["ACTT"])


    def even_mixer(self, l):
        ctx_out = (l < DEPTH - 1)
        self.ev_inproj(l)
        self.na_attn(l, ctx_out)
        self.mlstm_scan(l)
        self.mlstm_out(l, ctx_out)

    def ev_inproj(self, l):
        S = self.S
        e_ = l // 2
        with self.phase() as ph:
            W = ph.sb("Win", [128, 8, 3600], BF16)
            wv = self.wb["ev_w_in"][0][e_].rearrange("(k p) n -> p k n", p=128)
            for k in range(8):
                S.dma('sp', W[:, k, :], wv[:, k, :], reads=["wb"], writes=[W])
            cw = ph.sb("cw", [128, 3, 8], F32)
            cb = ph.sb("cb", [128, 8], F32)
            gb = ph.sb("gb", [128, 16], F32)
            S.dma('sp', cw[:], self.ev_conv_w[e_].rearrange("a (j p) -> p a j", p=128), writes=[cw], allow_slow_non_contiguous=True)
            S.dma('sp', cb[:], self.ev_conv_b[e_:e_ + 1, :].rearrange("o (j p) -> p (o j)", p=128), writes=[cb],
                  allow_slow_non_contiguous=True)
            S.dma('sp', gb[:], self.ev_gate_b[e_:e_ + 1, :].partition_broadcast(128), writes=[gb])
            hTs = Rot([ph.sb("hTi%d" % i, [128, 8, 514], BF16) for i in range(2)])
            pms = Rot([ph.ps("pm%d" % i, [128, 512]) for i in range(3)])
            hbank = ph.ps("hbank", [128, 512])
            phs = Rot([View(hbank[:, 8 * i:8 * i + 8], key=hbank) for i in range(8)])
            pts = Rot([ph.ps("pt%d" % i, [128, 512]) for i in range(3)])
            pg = ph.ps("pg", [128, 16])
            ubs = Rot([ph.sb("ub%d" % i, [128, 514], F32) for i in range(2)])
            accs = Rot([ph.sb("acc%d" % i, [128, 512], F32) for i in range(2)])
            sgs = Rot([ph.sb("sg%d" % i, [128, 512], F32) for i in range(2)])
            fms = Rot([ph.sb("fm%d" % i, [128, 16, 512], BF16) for i in range(2)])
            tms = Rot([ph.sb("tm%d" % i, [128, 4, 3, 512], BF16) for i in range(2)])
            gts = Rot([ph.sb("gt%d" % i, [128, 4, 16], F32) for i in range(2)])
            ges = Rot([ph.sb("ge%d" % i, [128, 2, 4], F32) for i in range(2)])
            qscale = 128 ** -0.5
            for HT, n, off in ((self.HTx, T, 0), (self.HTc, TC, T)):
                hv = HT.rearrange("(k p) t -> p k t", p=128)
                for tb in range(0, n, 512):
                    nb = min(512, n - tb)
                    hT = hTs.next()
                    S.dma('sp', hT[:, :, 0:nb + 2], hv[:, :, tb:tb + nb + 2], reads=["HT", "HTpad"], writes=[hT])
                    fm = fms.next()
                    for ch in range(8):
                        pm = pms.next()
                        S.op('pe', self.mm_halo(pm, None, W, hT, ch * 128, 128, nb), reads=[W, hT], writes=[pm])
                        S.op('act', lambda e: e.activation(out=fm[:, ch, 0:nb], in_=pm[:, 0:nb], func=AF.Copy), reads=[pm], writes=[fm])
                    for j in range(8):
                        pm = pms.next()
                        phl = phs.next()
                        S.op('pe', self.mm_halo(pm, phl, W, hT, 1536 + j * 128, 128, nb), reads=[W, hT], writes=[pm, phl])
                        ub = ubs.next()
                        S.op('act', lambda e: e.activation(out=ub[:, 1:nb + 1], in_=pm[:, 0:nb], func=AF.Copy), reads=[pm], writes=[ub])
                        S.op('act', lambda e: e.activation(out=ub[:, 0:nb + 2:nb + 1], in_=phl[:, 0:2], func=AF.Copy),
                             reads=[phl, ub], writes=[ub])
                        acc = accs.next()
                        self.conv3('dve', acc, ub, cw, cb, j, nb)
                        if j < 4:
                            sg = sgs.next()
                            S.op('act', lambda e: e.activation(out=sg[:, 0:nb], in_=acc[:, 0:nb], func=AF.Silu), reads=[acc], writes=[sg])
                            S.op('act', lambda e: e.activation(out=fm[:, 8 + j, 0:nb], in_=sg[:, 0:nb], func=AF.Copy, scale=qscale),
                                 reads=[sg], writes=[fm])
                        else:
                            S.op('act', lambda e: e.activation(out=fm[:, 8 + j, 0:nb], in_=acc[:, 0:nb], func=AF.Silu),
                                 reads=[acc], writes=[fm])
                    cs_ = slice(off + tb, off + tb + nb)
                    S.dma('sp', self.QTN.rearrange("(c p) t -> p c t", p=128)[:, :, cs_], fm[:, 0:4, 0:nb], reads=[fm], writes=["QTN"])
                    S.dma('sp', self.KTN.rearrange("(c p) t -> p c t", p=128)[:, :, cs_], fm[:, 4:8, 0:nb], reads=[fm], writes=["KTN"])
                    S.dma('sp', self.QKTM.rearrange("(c p) t -> p c t", p=128)[:, :, cs_], fm[:, 8:16, 0:nb], reads=[fm], writes=["QKTM"])
                    tm = tms.next()
                    gt = gts.next()
                    nj = nb // 128
                    for j in range(nj):
                        lhs = lambda k: hT[:, k, 1 + j * 128:1 + (j + 1) * 128]
                        for gi, c0 in enumerate((1024, 2560, 3072)):
                            pt = pts.next()
                            S.op('pe', [(lambda e, k=k: e.matmul(pt[:], lhsT=lhs(k), rhs=W[:, k, c0:c0 + 512], start=(k == 0), stop=(k == 7)))
                                        for k in range(8)], reads=[W, hT], writes=[pt])
                            if gi == 1:
                                S.op('dve', lambda e: e.tensor_copy(out=tm[:, j, gi, :], in_=pt[:]), reads=[pt], writes=[tm])
                            else:
                                S.op('act', lambda e: e.activation(out=tm[:, j, gi, :], in_=pt[:], func=AF.Copy), reads=[pt], writes=[tm])
                        S.op('pe', [(lambda e, k=k: e.matmul(pg[:], lhsT=lhs(k), rhs=W[:, k, 3584:3600], start=(k == 0), stop=(k == 7)))
                                    for k in range(8)], reads=[W, hT], writes=[pg])
                        S.op('dve', lambda e: e.tensor_tensor(out=gt[:, j, :], in0=pg[:], in1=gb[:], op=ALU.add), reads=[pg, gb], writes=[gt])
                        fv = gt[:, j, :].rearrange("p (a b) -> p a b", b=4)[:, 1:4:2, :]
                        ge = ges.next()
                        S.op('act', lambda e: e.activation(out=ge[:], in_=fv, func=AF.Exp, scale=-1.0), reads=[gt], writes=[ge])
                        S.op('dve', lambda e: e.tensor_scalar_add(out=ge[:], in0=ge[:], scalar1=1.0), reads=[ge], writes=[ge])
                        S.op('act', lambda e: e.activation(out=ge[:], in_=ge[:], func=AF.Ln), reads=[ge], writes=[ge])
                        S.op('dve', lambda e: e.tensor_scalar(out=fv, in0=ge[:], scalar1=-1.0, scalar2=None, op0=ALU.mult),
                             reads=[ge], writes=[gt])
                    rows = lambda Dd: Dd[off + tb: off + tb + nb, :].rearrange("(j p) f -> p j f", p=128)
                    S.dma('sp', rows(self.VN), tm[:, 0:nj, 0, :], reads=[tm], writes=["VN"])
                    S.dma('sp', rows(self.VML), tm[:, 0:nj, 1, :], reads=[tm], writes=["VML"])
                    S.dma('sp', rows(self.OML), tm[:, 0:nj, 2, :], reads=[tm], writes=["OML"])
                    S.dma('sp', rows(self.GML), gt[:, 0:nj, :], reads=[gt], writes=["GML"])

    def na_attn(self, l, ctx_out):
        S = self.S
        e_ = l // 2
        with self.phase() as ph:
            rp = ph.sb("rp", [120, 31], F32)
            Ep = ph.sb("Ep", [120, 128], F32)
            S.dma('sp', rp[:], self.ev_rpb[e_], writes=[rp])
            S.op('dve', lambda e: e.memset(Ep[:], 0.0), writes=[Ep])
            S.op('dve', lambda e: e.tensor_scalar(out=Ep[:, 0:16], in0=rp[:, 15:31], scalar1=8.0, scalar2=None, op0=ALU.mult),
                 reads=[rp, Ep], writes=[Ep])
            S.op('dve', lambda e: e.tensor_scalar(out=Ep[:, 113:128], in0=rp[:, 0:15], scalar1=8.0, scalar2=None, op0=ALU.mult),
                 reads=[rp, Ep], writes=[Ep])
            import os
            BTr = ph.sb("BTr", [64, 120, 64], F32)
            if os.environ.get("NA_SKIP") != "rb":
                S.dma('sp', self.RB.rearrange("a (r c) -> a r c", c=128), Ep[:].unsqueeze(1).broadcast_to([120, 64, 128]),
                      reads=[Ep], writes=["RB"])
                S.dma('sp', BTr[:], bass.AP(self.RB.tensor, 0, [[127, 64], [8192, 120], [1, 64]]), reads=["RB"], writes=[BTr])
            else:
                S.op('dve', lambda e: e.memset(BTr[:], 0.0), writes=[BTr])
            win = ph.sb("win", [64, 64], F32)
            S.dma('sp', win[:], self.k_win, writes=[win])
            BT = ph.sb("BT", [64, 120, 64], BF16)
            S.op('dve', lambda e: e.tensor_tensor(out=BT[:], in0=BTr[:], in1=win[:].unsqueeze(1).broadcast_to([64, 120, 64]), op=ALU.add),
                 reads=[BTr, win], writes=[BT])
            I64 = self.ident[0:64, 0:64]
            KTs = Rot([ph.sb("KTn%d" % i, [64, 2, TT], BF16) for i in range(2)])
            QTs = Rot([ph.sb("QTn%d" % i, [64, 2, TT], BF16) for i in range(2)])
            Vas = Rot([ph.sb("Va%d" % i, [128, 32, 2, 65], BF16) for i in range(2)])
            Vbs = Rot([ph.sb("Vb%d" % i, [128, 31, 2, 65], BF16) for i in range(2)])
            Vcs = Rot([ph.sb("Vc%d" % i, [128, 2, 2, 65], BF16) for i in range(2)])
            for v in Vas.tiles + Vbs.tiles + Vcs.tiles:
                S.op('dve', lambda e, v=v: e.memset(v[:, :, :, 64:65], 1.0), writes=[v])
            Sps = Rot([ph.ps("S%d" % i, [128, 6, 64]) for i in range(3)])
            Ops = Rot([ph.ps("O%d" % i, [64, 65]) for i in range(2)])
            Sc = ph.ps("Sc", [128, 2, 256])
            Ocs = [ph.ps("Oc%d" % i, [128, 65]) for i in range(2)]
            Ps = Rot([ph.sb("P%d" % i, [128, 6, 64], BF16) for i in range(3)])
            Pc = ph.sb("Pc", [128, 2, 256], BF16)
            rcs = Rot([ph.sb("rc%d" % i, [128, 1], F32) for i in range(4)])
            osts = Rot([ph.sb("ost%d" % i, [64, 8, 128], BF16) for i in range(2)])
            ostc = ph.sb("ostc", [128, 2, 128], BF16)
            ROWS = T // GRID
            import os
            NHP = int(os.environ.get('NA_NHP', '4'))
            for hp in range(NHP):
                KTt, QTt, Va, Vb, Vc = KTs.next(), QTs.next(), Vas.next(), Vbs.next(), Vcs.next()
                S.dma('sp', KTt[:], self.KTN[hp * 128:(hp + 1) * 128, :].rearrange("(h d) t -> d h t", d=64), reads=["KTN"], writes=[KTt])
                S.dma('sp', QTt[:], self.QTN[hp * 128:(hp + 1) * 128, :].rearrange("(h d) t -> d h t", d=64), reads=["QTN"], writes=[QTt])
                for hh in range(2):
                    c0 = hp * 128 + hh * 64
                    S.dma('sp', Va[:, :, hh, 0:64], self.VN[0:T, c0:c0 + 64].rearrange("(i p) f -> p i f", p=128), reads=["VN"], writes=[Va])
                    S.dma('sp', Vb[:, :, hh, 0:64], self.VN[64:64 + 31 * 128, c0:c0 + 64].rearrange("(i p) f -> p i f", p=128),
                          reads=["VN"], writes=[Vb])
                    S.dma('sp', Vc[:, :, hh, 0:64], self.VN[T:TT, c0:c0 + 64].rearrange("(i p) f -> p i f", p=128), reads=["VN"], writes=[Vc])
                for r in range(int(os.environ.get('NA_ROWS', str(ROWS)))):
                    rs = min(max(r - 4, 0), ROWS - 8)
                    if r % 8 == 0:
                        ost = osts.next()
                    for hh in range(int(os.environ.get('NA_HH', '2'))):
                        h = hp * 2 + hh
                        pr = slice(hh * 64, (hh + 1) * 64)
                        sp_ = Sps.next()
                        mms = []
                        for m in range(4):
                            tok0 = (rs + 2 * m) * 64
                            dr0 = rs + 2 * m - r + 7
                            mms.append(lambda e, m=m, tok0=tok0, hh=hh: e.matmul(sp_[:, m, :], lhsT=KTt[:, hh, tok0:tok0 + 128],
                                                                          rhs=QTt[:, hh, r * 64:(r + 1) * 64], start=True, stop=False))
                            mms.append(lambda e, m=m, dr0=dr0: e.matmul(sp_[:, m, :], lhsT=BT[:, h * 15 + dr0:h * 15 + dr0 + 2, :],
                                                                        rhs=I64, start=False, stop=True))
                        for m in range(2):
                            mms.append(lambda e, m=m, hh=hh: e.matmul(sp_[:, 4 + m, :], lhsT=KTt[:, hh, T + m * 128:T + (m + 1) * 128],
                                                               rhs=QTt[:, hh, r * 64:(r + 1) * 64], start=True, stop=True))
                        S.op('pe', mms, reads=[KTt, QTt, BT, self.ident], writes=[sp_])
                        P = Ps.next()
                        S.op('act', lambda e: e.activation(out=P[:], in_=sp_[:], func=AF.Exp, scale=0.125), reads=[sp_], writes=[P])
                        O = Ops.next()
                        mms = []
                        for m in range(6):
                            if m >= 4:
                                vv = Vc[:, m - 4, hh, :]
                            elif rs % 2 == 0:
                                vv = Va[:, (rs + 2 * m) // 2, hh, :]
                            else:
                                vv = Vb[:, (rs + 2 * m - 1) // 2, hh, :]
                            mms.append(lambda e, m=m, vv=vv: e.matmul(O[:], lhsT=P[:, m, :], rhs=vv, start=(m == 0), stop=(m == 5)))
                        S.op('pe', mms, reads=[P, Va, Vb, Vc], writes=[O])
                        rc = rcs.next()
                        S.op('dve', lambda e: e.reciprocal(out=rc[0:64, :], in_=O[:, 64:65]), reads=[O], writes=[rc])
                        S.op('dve', lambda e: e.tensor_scalar(out=ost[:, r % 8, pr], in0=O[:, 0:64], scalar1=rc[0:64, 0:1], scalar2=None,
                                                              op0=ALU.mult), reads=[O, rc], writes=[ost])
                    if r % 8 == 7:
                        r0 = r - 7
                        S.dma('sp', self.OMIX[r0 * 64:(r0 + 8) * 64, hp * 128:(hp + 1) * 128].rearrange("(rr p) f -> p rr f", p=64),
                              ost[:], reads=[ost], writes=["OMIX"])
                if ctx_out:
                    for hh in range(2):
                        pr = slice(hh * 64, (hh + 1) * 64)
                        S.op('pe', [(lambda e, kc=kc: e.matmul(Sc[:, kc, :], lhsT=KTt[:, hh, T + kc * 128:T + (kc + 1) * 128], rhs=QTt[:, hh, T:TT],
                                                               start=True, stop=True)) for kc in range(2)], reads=[KTt, QTt], writes=[Sc])
                        S.op('act', lambda e: e.activation(out=Pc[:], in_=Sc[:], func=AF.Exp, scale=0.125), reads=[Sc], writes=[Pc])
                        for qt in range(2):
                            O = Ocs[qt]
                            S.op('pe', [(lambda e, kc=kc: e.matmul(O[:], lhsT=Pc[:, kc, qt * 128:(qt + 1) * 128], rhs=Vc[:, kc, hh, :],
                                                                   start=(kc == 0), stop=(kc == 1))) for kc in range(2)],
                                 reads=[Pc, Vc], writes=[O])
                            rc = rcs.next()
                            S.op('dve', lambda e: e.reciprocal(out=rc[:], in_=O[:, 64:65]), reads=[O], writes=[rc])
                            S.op('dve', lambda e: e.tensor_scalar(out=ostc[:, qt, pr], in0=O[:, 0:64], scalar1=rc[:, 0:1], scalar2=None,
                                                                  op0=ALU.mult), reads=[O, rc], writes=[ostc])
                    S.dma('sp', self.OMIX[T:TT, hp * 128:(hp + 1) * 128].rearrange("(q p) f -> p q f", p=128), ostc[:],
                          reads=[ostc], writes=["OMIX"])

    def mlstm_scan(self, l):
        S = self.S
        with self.phase() as ph:
            tri = ph.sb("tri", [128, 4, 128], F32)
            S.dma('sp', tri[:], self.k_tri.rearrange("a p t -> p a t"), writes=[tri])
            NCH = TT // 128
            order = {0: [32, 33] + list(range(32)), 1: [33, 32] + list(range(31, -1, -1))}
            ST = [[ph.sb("ST%d_%d" % (d, h), [128, 129], F32) for h in range(4)] for d in range(2)]
            STb = [[ph.sb("STb%d_%d" % (d, h), [128, 129], BF16) for h in range(4)] for d in range(2)]
            for d in range(2):
                for h in range(4):
                    S.op('dve', lambda e, t=ST[d][h]: e.memset(t[:], 0.0), writes=[ST[d][h]])
                    S.op('dve', lambda e, t=STb[d][h]: e.memset(t[:], 0.0), writes=[STb[d][h]])
            QKs = Rot([ph.sb("QK%d" % i, [128, 8, 128], BF16) for i in range(4)])
            Vs = Rot([ph.sb("Vm%d" % i, [128, 4, 129], BF16) for i in range(4)])
            for v in Vs.tiles:
                S.op('dve', lambda e, v=v: e.memset(v[:, :, 128:129], 1.0), writes=[v])
            Gs = Rot([ph.sb("Gm%d" % i, [128, 16], F32) for i in range(4)])
            lfbs = Rot([ph.sb("lfb%d" % i, [128, 128], F32) for i in range(3)])
            css = Rot([ph.sb("cs%d" % i, [128, 4], F32) for i in range(6)])
            Dms = Rot([ph.sb("Dm%d" % i, [128, 128], F32) for i in range(3)])
            DTs = Rot([ph.sb("DT%d" % i, [128, 128], F32) for i in range(3)])
            abcs = Rot([ph.sb("abc%d" % i, [128, 128], F32) for i in range(3)])
            WTs = Rot([ph.sb("WT%d" % i, [128, 128], BF16) for i in range(3)])
            QaTs = Rot([ph.sb("QaT%d" % i, [128, 128], BF16) for i in range(3)])
            Kts = Rot([ph.sb("Kt%d" % i, [128, 128], BF16) for i in range(3)])
            Vws = Rot([ph.sb("Vw%d" % i, [128, 129], BF16) for i in range(3)])
            hsts = Rot([ph.sb("hst%d" % i, [128, 4, 128], F32) for i in range(4)])
            Bps = Rot([ph.ps("Bps%d" % i, [128, 129]) for i in range(2)])
            Sps = Rot([ph.ps("Sps%d" % i, [128, 128]) for i in range(2)])
            Hps = Rot([ph.ps("Hps%d" % i, [128, 129]) for i in range(2)])
            Tps = Rot([ph.ps("Tps%d" % i, [128, 128], BF16) for i in range(1)])
            KVps = Rot([ph.ps("KVps%d" % i, [128, 129]) for i in range(1)])
            qkv = self.QKTM.rearrange("(c p) t -> p c t", p=128)
            for step in range(NCH):
                for d in range(2):
                    ch = order[d][step]
                    cols = slice(ch * 128, (ch + 1) * 128)
                    QK, Vm, G = QKs.next(), Vs.next(), Gs.next()
                    S.dma('sp', QK[:], qkv[:, :, cols], reads=["QKTM"], writes=[QK])
                    S.dma('sp', Vm[:, :, 0:128], self.VML[cols, :].rearrange("p (h d) -> p h d", d=128), reads=["VML"], writes=[Vm])
                    S.dma('sp', G[:], self.GML[cols, :], reads=["GML"], writes=[G])
                    hst = hsts.next()
                    TRI = tri[:, d, :]
                    NEG = tri[:, 2 + d, :]
                    last = 127 if d == 0 else 0
                    for h in range(4):
                        icol = G[:, 8 * d + h:8 * d + h + 1]
                        lcol = G[:, 8 * d + 4 + h:8 * d + 4 + h + 1]
                        lfb = lfbs.next()
                        S.op('pool', lambda e: e.tensor_copy(out=lfb[:], in_=lcol.broadcast_to([128, 128])), reads=[G], writes=[lfb])
                        bp = Bps.next()
                        S.op('pe', [lambda e: e.matmul(bp[:, 0:128], lhsT=lfb[:], rhs=TRI, start=True, stop=True),
                                    lambda e: e.matmul(bp[:, 128:129], lhsT=TRI, rhs=lcol, start=True, stop=True)],
                             reads=[lfb, tri, G], writes=[bp])
                        cs = css.next()
                        S.op('dve', lambda e: e.tensor_tensor(out=cs[:, 0:1], in0=icol, in1=bp[:, 128:129], op=ALU.subtract),
                             reads=[G, bp], writes=[cs])
                        Dm = Dms.next()
                        S.op('dve', lambda e: e.tensor_tensor(out=Dm[:], in0=bp[:, 0:128], in1=NEG, op=ALU.add), reads=[bp, tri], writes=[Dm])
                        DT = DTs.next()
                        S.op('act', lambda e: e.activation(out=DT[:], in_=Dm[:], func=AF.Exp, bias=cs[:, 0:1]), reads=[Dm, cs], writes=[DT])
                        abc = abcs.next()
                        S.op('act', lambda e: e.activation(out=abc[:], in_=bp[:, 0:128], func=AF.Exp), reads=[bp], writes=[abc])
                        sp_ = Sps.next()
                        S.op('pe', lambda e: e.matmul(sp_[:], lhsT=QK[:, 4 + h, :], rhs=QK[:, h, :], start=True, stop=True),
                             reads=[QK], writes=[sp_])
                        WT = WTs.next()
                        S.op('dve', lambda e: e.tensor_tensor(out=WT[:], in0=sp_[:], in1=DT[:], op=ALU.mult), reads=[sp_, DT], writes=[WT])
                        QaT = QaTs.next()
                        S.op('pool', lambda e: e.tensor_tensor(out=QaT[:], in0=QK[:, h, :], in1=abc[:], op=ALU.mult),
                             reads=[QK, abc], writes=[QaT])
                        hp_ = Hps.next()
                        S.op('pe', [lambda e: e.matmul(hp_[:], lhsT=WT[:], rhs=Vm[:, h, :], start=True, stop=False),
                                    lambda e: e.matmul(hp_[:], lhsT=QaT[:], rhs=STb[d][h][:], start=False, stop=True)],
                             reads=[WT, Vm, QaT, STb[d][h]], writes=[hp_])
                        S.op('dve', lambda e: e.tensor_scalar(out=cs[:, 1:2], in0=hp_[:, 128:129], scalar1=-1.0, scalar2=None, op0=ALU.mult),
                             reads=[hp_, cs], writes=[cs])
                        S.op('dve', lambda e: e.tensor_tensor(out=cs[:, 1:2], in0=cs[:, 1:2], in1=hp_[:, 128:129], op=ALU.max),
                             reads=[hp_, cs], writes=[cs])
                        S.op('dve', lambda e: e.tensor_scalar_max(out=cs[:, 1:2], in0=cs[:, 1:2], scalar1=1.0), reads=[cs], writes=[cs])
                        S.op('dve', lambda e: e.reciprocal(out=cs[:, 2:3], in_=cs[:, 1:2]), reads=[cs], writes=[cs])
                        S.op('act', lambda e: e.activation(out=hst[:, h, :], in_=hp_[:, 0:128], func=AF.Copy, scale=cs[:, 2:3]),
                             reads=[hp_, cs], writes=[hst])
                        tp = Tps.next()
                        S.op('pe', lambda e: e.transpose(out=tp[:], in_=QK[:, 4 + h, :], identity=self.ident[:]),
                             reads=[QK, self.ident], writes=[tp])
                        Kt = Kts.next()
                        S.op('act', lambda e: e.activation(out=Kt[:], in_=tp[:], func=AF.Copy), reads=[tp], writes=[Kt])
                        Vw = Vws.next()
                        S.op('dve', lambda e: e.tensor_scalar(out=Vw[:], in0=Vm[:, h, :], scalar1=DT[:, last:last + 1], scalar2=None,
                                                              op0=ALU.mult), reads=[Vm, DT], writes=[Vw])
                        kv = KVps.next()
                        S.op('pe', lambda e: e.matmul(kv[:], lhsT=Kt[:], rhs=Vw[:], start=True, stop=True), reads=[Kt, Vw], writes=[kv])
                        st = ST[d][h]
                        S.op('dve', lambda e: e.scalar_tensor_tensor(out=st[:], in0=st[:], scalar=abc[:, last:last + 1], in1=kv[:],
                                                                     op0=ALU.mult, op1=ALU.add), reads=[st, abc, kv], writes=[st])
                        S.op('act', lambda e: e.activation(out=STb[d][h][:], in_=st[:], func=AF.Copy), reads=[st], writes=[STb[d][h]])
                    S.dma('sp', (self.HF if d == 0 else self.HB)[cols, :], hst[:].rearrange("p h d -> p (h d)"), reads=[hst],
                          writes=["HF" if d == 0 else "HB"])

    def mlstm_out(self, l, ctx_out):
        S = self.S
        e_ = l // 2
        with self.phase() as ph:
            g = ph.sb("mlg", [128, 512], F32)
            S.dma('sp', g[:], self.ev_ml_norm_g[e_:e_ + 1, :].partition_broadcast(128), writes=[g])
            hfs = Rot([ph.sb("hf%d" % i, [128, 512], F32) for i in range(2)])
            hbs = Rot([ph.sb("hb%d" % i, [128, 512], F32) for i in range(2)])
            obs = Rot([ph.sb("ob%d" % i, [128, 512], BF16) for i in range(2)])
            sgs = Rot([ph.sb("sg%d" % i, [128, 512], F32) for i in range(2)])
            t1s = Rot([ph.sb("t1%d" % i, [128, 512], F32) for i in range(2)])
            t2s = Rot([ph.sb("t2%d" % i, [128, 512], F32) for i in range(2)])
            sts = Rot([ph.sb("st%d" % i, [128, 3, 4], F32) for i in range(2)])
            outs = Rot([ph.sb("mo%d" % i, [128, 512], BF16) for i in range(2)])
            ntok = TT if ctx_out else T
            for t0 in range(0, ntok, 128):
                rows = slice(t0, t0 + 128)
                hf, hb, ob = hfs.next(), hbs.next(), obs.next()
                S.dma('sp', hf[:], self.HF[rows, :], reads=["HF"], writes=[hf])
                S.dma('sp', hb[:], self.HB[rows, :], reads=["HB"], writes=[hb])
                S.dma('sp', ob[:], self.OML[rows, :], reads=["OML"], writes=[ob])
                sg, t1, t2, st, mo = sgs.next(), t1s.next(), t2s.next(), sts.next(), outs.next()
                S.op('act', lambda e: e.activation(out=sg[:], in_=ob[:], func=AF.Sigmoid), reads=[ob], writes=[sg])
                S.op('pool', lambda e: e.tensor_tensor(out=t1[:], in0=hf[:], in1=hb[:], op=ALU.add), reads=[hf, hb], writes=[t1])
                S.op('dve', lambda e: e.tensor_tensor(out=t1[:], in0=t1[:], in1=sg[:], op=ALU.mult), reads=[t1, sg], writes=[t1])
                v1 = t1[:].rearrange("p (h d) -> p h d", d=128)
                v2 = t2[:].rearrange("p (h d) -> p h d", d=128)
                S.op('dve', lambda e: e.tensor_reduce(out=st[:, 0, :], in_=v1, axis=AX.X, op=ALU.add), reads=[t1], writes=[st])
                S.op('dve', lambda e: e.tensor_scalar(out=st[:, 0, :], in0=st[:, 0, :], scalar1=1.0 / 128, scalar2=None, op0=ALU.mult),
                     reads=[st], writes=[st])
                S.op('dve', lambda e: e.tensor_tensor(out=v1, in0=v1, in1=st[:, 0, :].unsqueeze(2).broadcast_to([128, 4, 128]),
                                                      op=ALU.subtract), reads=[t1, st], writes=[t1])
                S.op('pool', lambda e: e.tensor_tensor(out=t2[:], in0=t1[:], in1=t1[:], op=ALU.mult), reads=[t1], writes=[t2])
                S.op('dve', lambda e: e.tensor_reduce(out=st[:, 1, :], in_=v2, axis=AX.X, op=ALU.add), reads=[t2], writes=[st])
                S.op('dve', lambda e: e.tensor_scalar(out=st[:, 1, :], in0=st[:, 1, :], scalar1=1.0 / 128, scalar2=EPS,
                                                      op0=ALU.mult, op1=ALU.add), reads=[st], writes=[st])
                S.op('act', lambda e: e.activation(out=st[:, 1, :], in_=st[:, 1, :], func=AF.Sqrt), reads=[st], writes=[st])
                S.op('dve', lambda e: e.reciprocal(out=st[:, 2, :], in_=st[:, 1, :]), reads=[st], writes=[st])
                S.op('dve', lambda e: e.tensor_tensor(out=v2, in0=v1, in1=st[:, 2, :].unsqueeze(2).broadcast_to([128, 4, 128]),
                                                      op=ALU.mult), reads=[t1, st], writes=[t2])
                S.op('pool', lambda e: e.tensor_tensor(out=mo[:], in0=t2[:], in1=g[:], op=ALU.mult), reads=[t2, g], writes=[mo])
                S.dma('sp', self.OMIX[rows, 512:1024], mo[:], reads=[mo], writes=["OMIX"])


    def odd_mixer(self, l):
        self.mla_prep(l)
        self.mla_attn(l)

    def mla_prep(self, l):
        import os
        LVL = int(os.environ.get("PREP_LVL", "9"))
        S = self.S
        o = l // 2
        with self.phase() as ph:
            Wdq = ph.sb("Wdq", [128, 8, 256], BF16)
            Wdkv = ph.sb("Wdkv", [128, 8, 160], BF16)
            Wuq = ph.sb("Wuq", [128, 2, 1536], BF16)
            WukK = ph.sb("WukK", [128, 16, 64], BF16)
            WukV = ph.sb("WukV", [128, 16, 64], BF16)
            S.dma('sp', Wdq[:], self.wb["od_w_dq"][0][o].rearrange("(k p) n -> p k n", p=128), reads=["wb"], writes=[Wdq])
            S.dma('sp', Wdkv[:], self.wb["od_w_dkv"][0][o].rearrange("(k p) n -> p k n", p=128), reads=["wb"], writes=[Wdkv])
            S.dma('sp', Wuq[:], self.wb["od_w_uq"][0][o].rearrange("(k p) n -> p k n", p=128), reads=["wb"], writes=[Wuq])
            ukv = self.wb["od_w_ukv"][0][o].rearrange("p (h c) -> p h c", c=128)
            S.dma('sp', WukK[:], ukv[:, :, 0:64], reads=["wb"], writes=[WukK])
            S.dma('sp', WukV[:], ukv[:, :, 64:128], reads=["wb"], writes=[WukV])
            qg = ph.sb("qg", [128, 256], F32)
            kvg = ph.sb("kvg", [128, 128], F32)
            S.dma('sp', qg[:], self.od_q_norm_g[o:o + 1, :].partition_broadcast(128), writes=[qg])
            S.dma('sp', kvg[:], self.od_kv_norm_g[o:o + 1, :].partition_broadcast(128), writes=[kvg])
            b = {'junk': Rot([ph.sb("junk%d" % i, [128, 256], F32) for i in range(2)]),
                 'ss': Rot([ph.sb("ss%d" % i, [128, 2], F32) for i in range(6)])}
            hTs = Rot([ph.sb("hTi%d" % i, [128, 8, 512], BF16) for i in range(2)])
            pA = Rot([ph.ps("pA%d" % i, [128, 416]) for i in range(1)])
            pB = Rot([ph.ps("pB%d" % i, [128, 4, 128], BF16) for i in range(1)])
            pC = Rot([ph.ps("pC%d" % i, [128, 1536]) for i in range(1)])
            pD = Rot([ph.ps("pD%d" % i, [128, 12, 128], BF16) for i in range(1)])
            pE = Rot([ph.ps("pE%d" % i, [128, 512]) for i in range(1)])
            cqn = Rot([ph.sb("cqn%d" % i, [128, 256], BF16) for i in range(2)])
            ckvn = Rot([ph.sb("ckvn%d" % i, [128, 128], BF16) for i in range(2)])
            kpe = Rot([ph.sb("kpe%d" % i, [128, 128], BF16) for i in range(2)])
            for t_ in kpe.tiles:
                S.op('dve', lambda e, t_=t_: e.memset(t_[:], 0.0), writes=[t_])
            kpf = Rot([ph.sb("kpf%d" % i, [128, 32], F32) for i in range(2)])
            rt = Rot([ph.sb("rt%d" % i, [128, 16, 2, 8], F32) for i in range(4)])
            cqT = Rot([ph.sb("cqT%d" % i, [128, 2, 128], BF16) for i in range(2)])
            ckvT = Rot([ph.sb("ckvT%d" % i, [128, 512], BF16) for i in range(2)])
            kpeT = Rot([ph.sb("kpeT%d" % i, [128, 512], BF16) for i in range(2)])
            qtok = Rot([ph.sb("qtok%d" % i, [128, 16, 96], BF16) for i in range(2)])
            qfs = Rot([ph.sb("qf%d" % i, [128, 1536], F32) for i in range(2)])
            qTs = Rot([ph.sb("qTs%d" % i, [128, 12, 512], BF16) for i in range(2)])
            knT = Rot([ph.sb("knT%d" % i, [128, 512], BF16) for i in range(2)])
            vtk = Rot([ph.sb("vtk%d" % i, [128, 1024], BF16) for i in range(2)])
            for HT, n, off, rope in ((self.HTx, T, 0, True), (self.HTc, TC, T, False)):
                hv = HT.rearrange("(k p) t -> p k t", p=128)
                for tb in range(0, n, 512):
                    nb = min(512, n - tb)
                    hT = hTs.next()
                    S.dma('sp', hT[:, :, 0:nb], hv[:, :, 1 + tb:1 + tb + nb], reads=["HT"], writes=[hT])
                    ckT = ckvT.next()
                    kpT = kpeT.next()
                    qT = qTs.next()
                    for j in range(nb // 128):
                        tt = (tb // 128) + j
                        a = pA.next()
                        S.op('pe', [(lambda e, k=k: e.matmul(a[:, 0:256], lhsT=hT[:, k, j * 128:(j + 1) * 128], rhs=Wdq[:, k, :],
                                                             start=(k == 0), stop=(k == 7))) for k in range(8)] +
                             [(lambda e, k=k: e.matmul(a[:, 256:416], lhsT=hT[:, k, j * 128:(j + 1) * 128], rhs=Wdkv[:, k, :],
                                                       start=(k == 0), stop=(k == 7))) for k in range(8)],
                             reads=[hT, Wdq, Wdkv], writes=[a])
                        ssq = self.rstd_of(a[:, 0:256], a, b, 256)
                        cq = cqn.next()
                        S.op('dve', lambda e: e.scalar_tensor_tensor(out=cq[:], in0=a[:, 0:256], scalar=ssq[:, 1:2], in1=qg[:],
                                                                     op0=ALU.mult, op1=ALU.mult), reads=[a, ssq, qg], writes=[cq])
                        ssk = self.rstd_of(a[:, 256:384], a, b, 128)
                        ck = ckvn.next()
                        S.op('dve', lambda e: e.scalar_tensor_tensor(out=ck[:], in0=a[:, 256:384], scalar=ssk[:, 1:2], in1=kvg[:],
                                                                     op0=ALU.mult, op1=ALU.mult), reads=[a, ssk, kvg], writes=[ck])
                        kp = kpe.next()
                        if rope:
                            kf = kpf.next()
                            S.op('act', lambda e: e.activation(out=kf[:], in_=a[:, 384:416], func=AF.Copy), reads=[a], writes=[kf])
                            self.rope_tok(kf[:].rearrange("p (o a h j) -> p o a h j", o=1, a=2, h=2),
                                          kp[:, 0:32].rearrange("p (o a h j) -> p o a h j", o=1, a=2, h=2), 1, tt, rt, [kf], [kp])
                        else:
                            S.op('act', lambda e: e.activation(out=kp[:, 0:32], in_=a[:, 384:416], func=AF.Copy), reads=[a], writes=[kp])
                        if LVL < 1:
                            continue
                        tp = pB.next()
                        S.op('pe', [lambda e: e.transpose(out=tp[:, 0, :], in_=cq[:, 0:128], identity=self.ident[:]),
                                    lambda e: e.transpose(out=tp[:, 1, :], in_=cq[:, 128:256], identity=self.ident[:]),
                                    lambda e: e.transpose(out=tp[:, 2, :], in_=ck[:], identity=self.ident[:]),
                                    lambda e: e.transpose(out=tp[:, 3, :], in_=kp[:], identity=self.ident[:])],
                             reads=[cq, ck, kp, self.ident], writes=[tp])
                        cT = cqT.next()
                        S.op('act', lambda e: e.activation(out=cT[:], in_=tp[:, 0:2, :], func=AF.Copy), reads=[tp], writes=[cT])
                        S.op('act', lambda e: e.activation(out=ckT[:, j * 128:(j + 1) * 128], in_=tp[:, 2, :], func=AF.Copy),
                             reads=[tp], writes=[ckT])
                        S.op('act', lambda e: e.activation(out=kpT[:, j * 128:(j + 1) * 128], in_=tp[:, 3, :], func=AF.Copy),
                             reads=[tp], writes=[kpT])
                        if LVL < 2:
                            continue
                        cbig = pC.next()
                        S.op('pe', [(lambda e, k=k, nb_=nb_: e.matmul(cbig[:, nb_ * 512:(nb_ + 1) * 512], lhsT=cT[:, k, :],
                                                                      rhs=Wuq[:, k, nb_ * 512:(nb_ + 1) * 512],
                                                                      start=(k == 0), stop=(k == 1)))
                                    for nb_ in range(3) for k in range(2)], reads=[cT, Wuq], writes=[cbig])
                        qk = qtok.next()
                        qf = qfs.next()
                        for bk in range(3):
                            S.op('act', lambda e, bk=bk: e.activation(out=qf[:, bk * 512:(bk + 1) * 512], in_=cbig[:, bk * 512:(bk + 1) * 512],
                                                                      func=AF.Copy), reads=[cbig], writes=[qf])
                        qv = qf[:].rearrange("p (h c) -> p h c", c=96)
                        S.op('act', lambda e: e.activation(out=qk[:, :, 0:64], in_=qv[:, :, 0:64], func=AF.Copy),
                             reads=[qf], writes=[qk])
                        if rope:
                            self.rope_tok(qv[:, :, 64:96].rearrange("p h (a g j) -> p h a g j", a=2, g=2),
                                          qk[:, :, 64:96].rearrange("p h (a g j) -> p h a g j", a=2, g=2), 16, tt, rt,
                                          [qf], [qk])
                        else:
                            S.op('dve', lambda e: e.tensor_copy(out=qk[:, :, 64:96], in_=qv[:, :, 64:96]), reads=[qf], writes=[qk])
                        dT = pD.next()
                        qkf = qk[:].rearrange("p h c -> p (h c)")
                        S.op('pe', [(lambda e, c=c: e.transpose(out=dT[:, c, :], in_=qkf[:, c * 128:(c + 1) * 128], identity=self.ident[:]))
                                    for c in range(12)], reads=[qk, self.ident], writes=[dT])
                        S.op('act', lambda e: e.activation(out=qT[:, :, j * 128:(j + 1) * 128], in_=dT[:], func=AF.Copy),
                             reads=[dT], writes=[qT])
                        if LVL < 3:
                            continue
                        S.op('pe', [(lambda e, hf=hf: e.matmul(cbig[:, hf * 512:(hf + 1) * 512], lhsT=ckT[:, j * 128:(j + 1) * 128],
                                                               rhs=WukV[:, hf * 8:(hf + 1) * 8, :], start=True, stop=True))
                                    for hf in range(2)], reads=[ckT, WukV], writes=[cbig])
                        vt = vtk.next()
                        for bk in range(2):
                            S.op('dve', lambda e, bk=bk: e.tensor_copy(out=vt[:, bk * 512:(bk + 1) * 512], in_=cbig[:, bk * 512:(bk + 1) * 512]),
                                 reads=[cbig], writes=[vt])
                        S.dma('sp', self.VM[off + tb + j * 128: off + tb + (j + 1) * 128, :], vt[:], reads=[vt], writes=["VM"])
                    if LVL < 4:
                        continue
                    S.dma('sp', self.QT.rearrange("h f t -> (h f) t").rearrange("(c p) t -> p c t", p=128)[:, :, off + tb: off + tb + nb],
                          qT[:, :, 0:nb], reads=[qT], writes=["QT"])
                    if LVL < 5:
                        continue
                    for hp in range(8):
                        pe_ = pE.next()
                        S.op('pe', lambda e: e.matmul(pe_[:, 0:nb], lhsT=WukK[:, 2 * hp:2 * hp + 2, :], rhs=ckT[:, 0:nb],
                                                      start=True, stop=True), reads=[WukK, ckT], writes=[pe_])
                        kn = knT.next()
                        S.op('act', lambda e: e.activation(out=kn[:, 0:nb], in_=pe_[:, 0:nb], func=AF.Copy), reads=[pe_], writes=[kn])
                        for u in range(2):
                            S.dma('sp', self.KT[2 * hp + u, 0:64, off + tb: off + tb + nb], kn[u * 64:(u + 1) * 64, 0:nb],
                                  reads=[kn], writes=["KT"])
                    for h in range(16):
                        S.dma('sp', self.KT[h, 64:96, off + tb: off + tb + nb], kpT[0:32, 0:nb], reads=[kpT], writes=["KT"])

    def rope_tok(self, src, dst, nh, tt, rt, rk, wk):
        S = self.S
        cos = self.COS[:, tt, :].rearrange("p (a j) -> p a j", a=2).unsqueeze(1).broadcast_to([128, nh, 2, 8])
        sin = self.SIN[:, tt, :].rearrange("p (a j) -> p a j", a=2).unsqueeze(1).broadcast_to([128, nh, 2, 8])
        x1 = src[:, :, :, 0, :]
        x2 = src[:, :, :, 1, :]
        t = [rt.next() for _ in range(4)]
        tv = [q[:, 0:nh, :, :] for q in t]
        S.op('dve', lambda e: e.tensor_tensor(out=tv[0], in0=x1, in1=cos, op=ALU.mult), reads=rk + [self.COS], writes=[t[0]])
        S.op('dve', lambda e: e.tensor_tensor(out=tv[1], in0=x2, in1=sin, op=ALU.mult), reads=rk + [self.SIN], writes=[t[1]])
        S.op('dve', lambda e: e.tensor_tensor(out=tv[2], in0=x1, in1=sin, op=ALU.mult), reads=rk + [self.SIN], writes=[t[2]])
        S.op('dve', lambda e: e.tensor_tensor(out=tv[3], in0=x2, in1=cos, op=ALU.mult), reads=rk + [self.COS], writes=[t[3]])
        S.op('dve', lambda e: e.tensor_tensor(out=dst[:, :, :, 0, :], in0=tv[0], in1=tv[1], op=ALU.subtract),
             reads=[t[0], t[1]], writes=wk)
        S.op('dve', lambda e: e.tensor_tensor(out=dst[:, :, :, 1, :], in0=tv[2], in1=tv[3], op=ALU.add),
             reads=[t[2], t[3]], writes=wk)

    def mla_attn(self, l):
        S = self.S
        NKC = TT // 128
        scale = 96 ** -0.5
        with self.phase() as ph:
            KTs = Rot([ph.sb("KTs%d" % i, [128, 4, TT], BF16) for i in range(2)])
            for t_ in KTs.tiles:
                S.op('dve', lambda e, t_=t_: e.memset(t_[:], 0.0), writes=[t_])
            Vs = Rot([ph.sb("Vs%d" % i, [128, NKC, 4, 65], BF16) for i in range(2)])
            for v in Vs.tiles:
                S.op('dve', lambda e, v=v: e.memset(v[:, :, :, 64:65], 1.0), writes=[v])
            QTs = Rot([ph.sb("QTb%d" % i, [128, 4, 512], BF16) for i in range(2)])
            for t_ in QTs.tiles:
                S.op('dve', lambda e, t_=t_: e.memset(t_[:], 0.0), writes=[t_])
            Ps = Rot([ph.sb("P%d" % i, [128, 512], BF16) for i in range(4)])
            Sps = Rot([ph.ps("S%d" % i, [128, 512]) for i in range(4)])
            Obk = [ph.ps("O%d" % i, [128, 65]) for i in range(4)]
            rcs = Rot([ph.sb("rc%d" % i, [128, 4], F32) for i in range(4)])
            Ost = Rot([ph.sb("Ost%d" % i, [128, 4, 4, 64], BF16) for i in range(2)])
            ctx_out = (l < DEPTH - 1)
            qblocks = [(qb * 512, 512, list(range(NKC))) for qb in range(T // 512)]
            if ctx_out:
                qblocks.append((T, TC, [NKC - 2, NKC - 1]))
            for hg in range(4):
                KTt = KTs.next()
                Vt = Vs.next()
                S.dma('sp', KTt[0:96, :, :], self.KT.rearrange("h f t -> f h t")[:, hg * 4:(hg + 1) * 4, :], reads=["KT"], writes=[KTt])
                vsrc = self.VM.rearrange("(kc p) (h d) -> p kc h d", p=128, d=64)
                for h in range(4):
                    S.dma('sp', Vt[:, :, h, 0:64], vsrc[:, :, hg * 4 + h, :], reads=["VM"], writes=[Vt])
                for q0, nq, kcs in qblocks:
                    Qt = QTs.next()
                    S.dma('sp', Qt[0:96, :, 0:nq], self.QT.rearrange("h f t -> f h t")[:, hg * 4:(hg + 1) * 4, q0:q0 + nq],
                          reads=["QT"], writes=[Qt])
                    ost = Ost.next()
                    nqt = nq // 128
                    for h in range(4):
                        for ci, kc in enumerate(kcs):
                            sp_ = Sps.next()
                            S.op('pe', lambda e: e.matmul(sp_[:, 0:nq], lhsT=KTt[:, h, kc * 128:(kc + 1) * 128], rhs=Qt[:, h, 0:nq],
                                                          start=True, stop=True), reads=[KTt, Qt], writes=[sp_])
                            P = Ps.next()
                            S.op('act', lambda e: e.activation(out=P[:, 0:nq], in_=sp_[:, 0:nq], func=AF.Exp, scale=scale),
                                 reads=[sp_], writes=[P])
                            S.op('pe', [(lambda e, qt=qt: e.matmul(Obk[qt][:], lhsT=P[:, qt * 128:(qt + 1) * 128], rhs=Vt[:, kc, h, :],
                                                                   start=(ci == 0), stop=(ci == len(kcs) - 1)))
                                        for qt in range(nqt)], reads=[P, Vt], writes=Obk[0:nqt])
                        rc = rcs.next()
                        for qt in range(nqt):
                            O = Obk[qt]
                            S.op('dve', lambda e: e.reciprocal(out=rc[:, qt:qt + 1], in_=O[:, 64:65]), reads=[O], writes=[rc])
                            S.op('dve', lambda e: e.tensor_scalar(out=ost[:, qt, h, :], in0=O[:, 0:64], scalar1=rc[:, qt:qt + 1],
                                                                  scalar2=None, op0=ALU.mult), reads=[O, rc], writes=[ost])
                    S.dma('sp', self.OMIX[q0:q0 + nq, hg * 256:(hg + 1) * 256].rearrange("(qt p) f -> p qt f", p=128),
                          ost[:, 0:nqt, :, :].rearrange("p q h d -> p q (h d)"), reads=[ost], writes=["OMIX"])


def host_consts():
    ident = np.eye(128, dtype=np.float32)
    U = np.triu(np.ones((128, 128), np.float32))
    L = U.T.copy()
    tri = np.stack([U, L, (U - 1.0) * 1e4, (L - 1.0) * 1e4]).astype(np.float32)
    pos = np.arange(T)
    row = (pos // GRID).astype(np.float32)
    col = (pos % GRID).astype(np.float32)
    kpos = np.concatenate([np.repeat(row[:, None], 8, 1), np.repeat(col[:, None], 8, 1)], axis=1).astype(np.float32)
    inv = (10000.0 ** (-(np.arange(8, dtype=np.float32)) / 8.0)).astype(np.float32)
    kinv = np.concatenate([inv, inv])[None, :].astype(np.float32)
    qc = np.arange(64)
    cs = np.clip(qc - 8, 0, 48)
    win = ((qc[None, :] >= cs[:, None]) & (qc[None, :] < cs[:, None] + 16))
    kwin = np.where(win, 0.0, -30000.0).astype(np.float32)
    return dict(k_ident=ident, k_tri=tri, k_pos=kpos, k_inv=kinv, k_win=kwin)


def make_in_maps(inputs, cores):
    hc = host_consts()
    f = lambda a: np.ascontiguousarray(np.asarray(a, dtype=np.float32))
    shared = {}
    for name in ("ada_w", "ffn_w_up", "ffn_conv_w", "ffn_conv_b", "ffn_w_down", "ev_w_in", "ev_gate_b",
                 "ev_conv_w", "ev_conv_b", "ev_ml_norm_g", "ev_w_out", "od_w_dq", "od_q_norm_g", "od_w_uq",
                 "od_w_dkv", "od_kv_norm_g", "od_w_ukv", "od_w_o", "ada_b"):
        shared[name] = f(inputs[name])
    shared["norm_g"] = f(inputs["norm_g"]).reshape(DEPTH, 4 * D)
    shared["ev_rpb"] = f(inputs["ev_rpb"]).reshape(2, 120, 31)
    shared["c_ctx"] = f(inputs["c_ctx"]).reshape(1, D)
    shared.update(hc)
    maps = []
    for b in cores:
        m = dict(shared)
        m["x"] = f(inputs["x"][b])
        m["c"] = f(inputs["c"][b]).reshape(1, D)
        m["ctx"] = f(inputs["ctx"][b])
        maps.append(m)
    return maps


def kernel(**inputs):
    kb = K()
    nc = kb.build()
    maps = make_in_maps(inputs, list(range(8)))
    res = run_bass_kernel_spmd(nc, maps, core_ids=list(range(8)))
    return np.stack([np.asarray(r["out"], dtype=np.float32) for r in res.results], axis=0)
```

```python
import numpy as np
from contextlib import ExitStack, contextmanager
import concourse.bass as bass
import concourse.mybir as mybir
from concourse.bass_utils import run_bass_kernel_spmd

F32 = mybir.dt.float32
BF16 = mybir.dt.bfloat16
AF = mybir.ActivationFunctionType
ALU = mybir.AluOpType
AX = mybir.AxisListType

T = 4096
TC = 256
TT = T + TC
D = 1024
DEPTH = 4
GRID = 64
EPS = 1e-6
FH = 2816
NDMA = 40


class Tl:
    def __init__(self, h, name):
        self.h = h
        self.name = name

    def __getitem__(self, k):
        return self.h[k]


class Sched:
    def __init__(self, nc, es):
        self.nc = nc
        self.engs = {'pe': nc.tensor, 'dve': nc.vector, 'act': nc.scalar, 'pool': nc.gpsimd, 'sp': nc.sync}
        self.sems = {e: es.enter_context(nc.semaphore("s_" + e)) for e in ('pe', 'dve', 'act', 'pool')}
        self.cnt = {e: 0 for e in self.sems}
        self.known = {e: {} for e in self.engs}
        self.dsems = [es.enter_context(nc.semaphore("d%d" % i)) for i in range(NDMA)]
        self.dval = [0] * NDMA
        self.dnext = 0
        self.dnext_sw = NDMA - 12
        self.lastw = {}
        self.readers = {}
        self.uid = 0
        self.nins = 0

    def _wait(self, e, ev):
        clk, val, snap = ev
        if clk == e and e == 'pe':
            return
        kn = self.known[e]
        if kn.get(clk, 0) >= val:
            return
        sem = self.sems[clk] if isinstance(clk, str) else self.dsems[clk]
        self.engs[e].wait_ge(sem, val)
        self.nins += 1
        new = dict(kn)
        new[clk] = val
        for c_, v_ in snap.items():
            if new.get(c_, 0) < v_:
                new[c_] = v_
        self.known[e] = new

    def _deps(self, e, reads, writes):
        for k in reads:
            ev = self.lastw.get(k)
            if ev is not None:
                self._wait(e, ev)
        for k in writes:
            ev = self.lastw.get(k)
            if ev is not None:
                self._wait(e, ev)
            rd = self.readers.get(k)
            if rd:
                for clk, (val, snap) in rd.items():
                    self._wait(e, (clk, val, snap))

    def _commit(self, ev, reads, writes):
        clk, val, snap = ev
        for k in reads:
            self.readers.setdefault(k, {})[clk] = (val, snap)
        for k in writes:
            self.lastw[k] = ev
            self.readers[k] = {}

    def op(self, e, fns, reads=(), writes=()):
        reads = [getattr(k, 'key', k) for k in reads]
        writes = [getattr(k, 'key', k) for k in writes]
        self._deps(e, reads, writes)
        if callable(fns):
            fns = [fns]
        eng = self.engs[e]
        for f in fns[:-1]:
            f(eng)
        ins = fns[-1](eng)
        self.nins += len(fns)
        self.cnt[e] += 1
        ins.then_inc(self.sems[e], 1)
        self._commit((e, self.cnt[e], self.known[e]), reads, writes)

    def dma(self, q, out, in_, reads=(), writes=(), **kw):
        if q == 'pool':
            i = self.dnext_sw
            self.dnext_sw = NDMA - 12 + (i + 1 - (NDMA - 12)) % 12
        else:
            i = self.dnext
            self.dnext = (i + 1) % (NDMA - 12)
        if self.dval[i] > 0:
            self._wait(q, (i, self.dval[i], {}))
        self._deps(q, reads, writes)
        self.engs[q].dma_start(out=out, in_=in_, **kw).then_inc(self.dsems[i], 16)
        self.nins += 1
        self.dval[i] += 16
        self._commit((i, self.dval[i], self.known[q]), reads, writes)

    def barrier(self):
        for e in self.engs:
            for c_ in self.cnt:
                if self.cnt[c_] > 0:
                    self._wait(e, (c_, self.cnt[c_], {}))
            for i in range(NDMA):
                if self.dval[i] > 0:
                    self._wait(e, (i, self.dval[i], {}))
        self.lastw = {}
        self.readers = {}


class Phase:
    def __init__(self, K):
        self.K = K
        self.es = ExitStack()

    def sb(self, name, shape, dt=F32):
        self.K.S.uid += 1
        h = self.es.enter_context(self.K.nc.sbuf_tensor("%s_%d" % (name, self.K.S.uid), list(shape), dt))
        return Tl(h, name)

    def ps(self, name, shape, dt=F32):
        self.K.S.uid += 1
        h = self.es.enter_context(self.K.nc.psum_tensor("%s_%d" % (name, self.K.S.uid), list(shape), dt))
        return Tl(h, name)


class View:
    def __init__(self, ap, key=None, name="view"):
        self.ap = ap
        self.name = name
        self.key = key if key is not None else self

    def __getitem__(self, k):
        return self.ap[k]


class Rot:
    def __init__(self, tiles):
        self.tiles = tiles
        self.i = 0

    def next(self):
        t = self.tiles[self.i % len(self.tiles)]
        self.i += 1
        return t


class K:
    def __init__(self, dbg=(), nlayers=DEPTH, upto=None):
        self.dbg = set(dbg)
        self.nlayers = nlayers
        self.upto = upto
        self.nc = bass.Bass("TRN2", target_bir_lowering=False)
        self.es = ExitStack()

    def din(self, name, shape):
        return self.nc.dram_tensor(name, list(shape), F32, kind="ExternalInput").ap()

    def dscr(self, name, shape, dt=BF16):
        kind = "ExternalOutput" if name in self.dbg else "Internal"
        return self.nc.dram_tensor(name, list(shape), dt, kind=kind).ap()

    @contextmanager
    def phase(self):
        ph = Phase(self)
        with ph.es:
            yield ph
            self.S.barrier()

    def build(self):
        nc = self.nc
        with self.es:
            self.S = Sched(nc, self.es)
            self.declare()
            self.consts()
            self.cast_weights()
            self.ada_phase()
            if getattr(self, 'plan', None) is not None:
                self.plan(self)
                self.S.barrier()
                return nc
            x_in, xc_in = self.x, self.ctx
            self.norm_only(0, x_in, xc_in)
            for l in range(self.nlayers):
                last = (l == DEPTH - 1)
                if l % 2 == 0:
                    self.even_mixer(l)
                else:
                    self.odd_mixer(l)
                if self.upto == ('mixer', l):
                    break
                self.out_phase(l, 'mix', x_in, xc_in, self.X, self.XC, ctx_out=not last)
                x_in, xc_in = self.X, self.XC
                if self.upto == ('mixout', l):
                    break
                self.ffn1(l, ctx_out=not last)
                if self.upto == ('ffn1', l):
                    break
                self.out_phase(l, 'ffn', x_in, xc_in, (self.out if last else self.X), self.XC, ctx_out=not last)
            self.S.barrier()
        return nc

    def declare(self):
        din = self.din
        self.x = din("x", [T, D])
        self.c = din("c", [1, D])
        self.ctx = din("ctx", [TC, D])
        self.c_ctx = din("c_ctx", [1, D])
        self.ada_w = din("ada_w", [DEPTH, D, 6 * D])
        self.ada_b = din("ada_b", [DEPTH, 6 * D])
        self.norm_g = din("norm_g", [DEPTH, 4 * D])
        self.ffn_w_up = din("ffn_w_up", [DEPTH, D, 2 * FH])
        self.ffn_conv_w = din("ffn_conv_w", [DEPTH, 3, 2 * FH])
        self.ffn_conv_b = din("ffn_conv_b", [DEPTH, 2 * FH])
        self.ffn_w_down = din("ffn_w_down", [DEPTH, FH, D])
        self.ev_w_in = din("ev_w_in", [2, D, 3600])
        self.ev_gate_b = din("ev_gate_b", [2, 16])
        self.ev_conv_w = din("ev_conv_w", [2, 3, 1024])
        self.ev_conv_b = din("ev_conv_b", [2, 1024])
        self.ev_rpb = din("ev_rpb", [2, 120, 31])
        self.ev_ml_norm_g = din("ev_ml_norm_g", [2, 512])
        self.ev_w_out = din("ev_w_out", [2, D, D])
        self.od_w_dq = din("od_w_dq", [2, D, 256])
        self.od_q_norm_g = din("od_q_norm_g", [2, 256])
        self.od_w_uq = din("od_w_uq", [2, 256, 1536])
        self.od_w_dkv = din("od_w_dkv", [2, D, 160])
        self.od_kv_norm_g = din("od_kv_norm_g", [2, 128])
        self.od_w_ukv = din("od_w_ukv", [2, 128, 2048])
        self.od_w_o = din("od_w_o", [2, D, D])
        self.k_ident = din("k_ident", [128, 128])
        self.k_tri = din("k_tri", [4, 128, 128])
        self.k_pos = din("k_pos", [T, 16])
        self.k_inv = din("k_inv", [1, 16])
        self.k_win = din("k_win", [64, 64])
        self.out = self.nc.dram_tensor("out", [T, D], F32, kind="ExternalOutput").ap()
        ds = self.dscr
        self.X = ds("X", [T, D], F32)
        self.wb = {}
        for nm, shp in (("ffn_w_up", [DEPTH, D, 2 * FH]), ("ffn_w_down", [DEPTH, FH, D]), ("ev_w_in", [2, D, 3600]),
                        ("ev_w_out", [2, D, D]), ("od_w_dq", [2, D, 256]), ("od_w_uq", [2, 256, 1536]),
                        ("od_w_dkv", [2, D, 160]), ("od_w_ukv", [2, 128, 2048]), ("od_w_o", [2, D, D])):
            self.wb[nm] = (ds("wb_" + nm, shp), shp)
        self.XC = ds("XC", [TC, D], F32)
        self.VEC = ds("VEC", [DEPTH, 2, 6 * D], F32)
        self.HTx = ds("HTx", [D, T + 2])
        self.HTc = ds("HTc", [D, TC + 2])
        self.ACTT = ds("ACTT", [FH, TT])
        self.OMIX = ds("OMIX", [TT, D])
        self.QTN = ds("QTN", [512, TT])
        self.KTN = ds("KTN", [512, TT])
        self.VN = ds("VN", [TT, 512])
        self.QKTM = ds("QKTM", [1024, TT])
        self.VML = ds("VML", [TT, 512])
        self.OML = ds("OML", [TT, 512])
        self.GML = ds("GML", [TT, 16], F32)
        self.HF = ds("HF", [TT, 512], F32)
        self.HB = ds("HB", [TT, 512], F32)
        self.RB = ds("RB", [120, 8192], F32)
        self.QT = ds("QT", [16, 96, TT])
        self.KT = ds("KT", [16, 96, TT])
        self.VM = ds("VM", [TT, D])

    def consts(self):
        S = self.S
        ph = Phase(self)
        self.cph = ph
        self.es.enter_context(ph.es)
        self.ident = ph.sb("ident", [128, 128], BF16)
        S.dma('pool', self.ident[:], self.k_ident, writes=[self.ident])
        self.zcol = ph.sb("zcol", [128, 8], BF16)
        S.op('pool', lambda e: e.memset(self.zcol[:], 0.0), writes=[self.zcol])
        for HT, n in ((self.HTx, T), (self.HTc, TC)):
            v = HT.rearrange("(k p) t -> p k t", p=128)
            S.dma('sp', v[:, :, 0:1], self.zcol[:].rearrange("p (k o) -> p k o", o=1), reads=[self.zcol],
                  writes=["HTpad"], allow_slow_non_contiguous=True)
            S.dma('sp', v[:, :, n + 1:n + 2], self.zcol[:].rearrange("p (k o) -> p k o", o=1), reads=[self.zcol],
                  writes=["HTpad"], allow_slow_non_contiguous=True)
        import math
        pos = ph.sb("pos", [128, 32, 16], F32)
        inv = ph.sb("inv", [128, 16], F32)
        ang = ph.sb("ang", [128, 32, 16], F32)
        self.COS = ph.sb("COS", [128, 32, 16], F32)
        self.SIN = ph.sb("SIN", [128, 32, 16], F32)
        S.dma('sp', pos[:], self.k_pos.rearrange("(n p) j -> p n j", p=128), writes=[pos], allow_slow_non_contiguous=True)
        S.dma('sp', inv[:], self.k_inv.partition_broadcast(128), writes=[inv])
        S.op('dve', lambda e: e.tensor_tensor(out=ang[:], in0=pos[:], in1=inv[:].unsqueeze(1).broadcast_to([128, 32, 16]),
                                              op=ALU.mult), reads=[pos, inv], writes=[ang])
        kacc = ph.sb("kacc", [128, 32, 16], F32)
        for dst, shift in ((self.SIN, 0.5), (self.COS, 0.75)):
            S.op('dve', lambda e: e.tensor_scalar(out=pos[:], in0=ang[:], scalar1=1.0 / (2 * math.pi), scalar2=shift,
                                                  op0=ALU.mult, op1=ALU.add), reads=[ang], writes=[pos])
            S.op('dve', lambda e: e.memset(kacc[:], 0.5), writes=[kacc])
            for m in range(1, 13):
                S.op('dve', lambda e, m=m: e.scalar_tensor_tensor(out=kacc[:], in0=pos[:], scalar=float(m), in1=kacc[:],
                                                                  op0=ALU.is_ge, op1=ALU.add), reads=[pos, kacc], writes=[kacc])
            S.op('dve', lambda e: e.tensor_tensor(out=pos[:], in0=pos[:], in1=kacc[:], op=ALU.subtract),
                 reads=[pos, kacc], writes=[pos])
            S.op('act', lambda e: e.activation(out=dst[:], in_=pos[:], func=AF.Sin, scale=2 * math.pi), reads=[pos], writes=[dst])
        S.barrier()

    def cast_weights(self):
        S = self.S
        for nm, (dst, shp) in self.wb.items():
            src = getattr(self, nm)
            for i in range(shp[0]):
                if nm.startswith("ev_") and 2 * i >= self.nlayers:
                    continue
                if nm.startswith("od_") and 2 * i + 1 >= self.nlayers:
                    continue
                if nm.startswith("ffn_") and i >= self.nlayers:
                    continue
                for r0 in range(0, shp[1], 256):
                    r1 = min(shp[1], r0 + 256)
                    S.dma('pool', dst[i, r0:r1, :], src[i, r0:r1, :], writes=["wb"])
        S.barrier()

    def ada_phase(self):
        S = self.S
        with self.phase() as ph:
            cT = ph.sb("cT", [128, 8, 2], F32)
            cs = ph.sb("cs", [128, 8, 2], BF16)
            S.dma('sp', cT[:, :, 0], self.c.rearrange("o (k p) -> p (o k)", p=128), writes=[cT],
                  allow_slow_non_contiguous=True)
            S.dma('sp', cT[:, :, 1], self.c_ctx.rearrange("o (k p) -> p (o k)", p=128), writes=[cT],
                  allow_slow_non_contiguous=True)
            S.op('act', lambda e: e.activation(out=cs[:], in_=cT[:], func=AF.Silu), reads=[cT], writes=[cs])
            wts = Rot([ph.sb("adaw%d" % i, [128, 8, 512], BF16) for i in range(3)])
            pss = Rot([ph.ps("adaps%d" % i, [2, 512]) for i in range(2)])
            MOD = ph.sb("MOD", [2, 6 * D], F32)
            ADB = ph.sb("ADB", [2, 6 * D], F32)
            NG = ph.sb("NG", [2, 4 * D], F32)
            VEC = ph.sb("VECs", [2, 6 * D], F32)
            for l in range(self.nlayers):
                S.dma('sp', ADB[:], self.ada_b[l:l + 1, :].partition_broadcast(2), writes=[ADB])
                S.dma('sp', NG[:], self.norm_g[l:l + 1, :].partition_broadcast(2), writes=[NG])
                for nb in range(12):
                    wt = wts.next()
                    S.dma('pool', wt[:], self.ada_w[l][:, nb * 512:(nb + 1) * 512].rearrange("(k p) n -> p k n", p=128),
                          writes=[wt])
                    pt = pss.next()
                    S.op('pe', [(lambda e, k=k: e.matmul(pt[:], lhsT=cs[:, k, :], rhs=wt[:, k, :],
                                                         start=(k == 0), stop=(k == 7))) for k in range(8)],
                         reads=[cs, wt], writes=[pt])
                    S.op('dve', lambda e: e.tensor_tensor(out=MOD[:, nb * 512:(nb + 1) * 512], in0=pt[:],
                                                          in1=ADB[:, nb * 512:(nb + 1) * 512], op=ALU.add),
                         reads=[pt, ADB], writes=[MOD])

                def sl(t, i):
                    return t[:, i * D:(i + 1) * D]
                S.op('dve', lambda e: e.scalar_tensor_tensor(out=sl(VEC, 0), in0=sl(MOD, 1), scalar=1.0, in1=sl(NG, 0),
                                                             op0=ALU.add, op1=ALU.mult), reads=[MOD, NG], writes=[VEC])
                S.op('dve', lambda e: e.tensor_copy(out=sl(VEC, 1), in_=sl(MOD, 0)), reads=[MOD], writes=[VEC])
                S.op('dve', lambda e: e.tensor_tensor(out=sl(VEC, 2), in0=sl(MOD, 2), in1=sl(NG, 1), op=ALU.mult),
                     reads=[MOD, NG], writes=[VEC])
                S.op('dve', lambda e: e.scalar_tensor_tensor(out=sl(VEC, 3), in0=sl(MOD, 4), scalar=1.0, in1=sl(NG, 2),
                                                             op0=ALU.add, op1=ALU.mult), reads=[MOD, NG], writes=[VEC])
                S.op('dve', lambda e: e.tensor_copy(out=sl(VEC, 4), in_=sl(MOD, 3)), reads=[MOD], writes=[VEC])
                S.op('dve', lambda e: e.tensor_tensor(out=sl(VEC, 5), in0=sl(MOD, 5), in1=sl(NG, 3), op=ALU.mult),
                     reads=[MOD, NG], writes=[VEC])
                S.dma('sp', self.VEC[l], VEC[:], reads=[VEC], writes=["VEC"])

    def load_vec(self, ph, l, which, idx, name):
        t = ph.sb(name, [128, D], F32)
        self.S.dma('sp', t[:], self.VEC[l, which:which + 1, idx * D:(idx + 1) * D].partition_broadcast(128),
                   reads=["VEC"], writes=[t])
        return t

    def mk_norm_bufs(self, ph, n=2):
        b = {}
        b['junk'] = Rot([ph.sb("junk%d" % i, [128, D], F32) for i in range(n)])
        b['ss'] = Rot([ph.sb("ss%d" % i, [128, 2], F32) for i in range(2 * n)])
        b['tmp'] = Rot([ph.sb("tmp%d" % i, [128, D], F32) for i in range(n)])
        b['hb'] = Rot([ph.sb("hb%d" % i, [128, D], BF16) for i in range(n)])
        b['tp'] = Rot([ph.ps("tp%d" % i, [128, 8, 128], BF16) for i in range(2)])
        b['hT'] = Rot([ph.sb("hT%d" % i, [128, 8, 512], BF16) for i in range(2)])
        return b

    def rstd_of(self, src, srck, b, n):
        S = self.S
        junk = b['junk'].next()
        ss = b['ss'].next()
        S.op('act', lambda e: e.activation(out=junk[:, 0:n], in_=src, func=AF.Square, accum_out=ss[:, 0:1]),
             reads=[srck], writes=[junk, ss])
        S.op('dve', lambda e: e.tensor_scalar(out=ss[:, 1:2], in0=ss[:, 0:1], scalar1=1.0 / n, scalar2=EPS,
                                              op0=ALU.mult, op1=ALU.add), reads=[ss], writes=[ss])
        S.op('act', lambda e: e.activation(out=ss[:, 1:2], in_=ss[:, 1:2], func=AF.Sqrt), reads=[ss], writes=[ss])
        S.op('dve', lambda e: e.reciprocal(out=ss[:, 1:2], in_=ss[:, 1:2]), reads=[ss], writes=[ss])
        return ss

    def norm_to_hT(self, xt, A, SH, b, hT, col):
        S = self.S
        ss = self.rstd_of(xt[:], xt, b, D)
        tmp = b['tmp'].next()
        hb = b['hb'].next()
        S.op('dve', lambda e: e.scalar_tensor_tensor(out=tmp[:], in0=xt[:], scalar=ss[:, 1:2], in1=A[:],
                                                     op0=ALU.mult, op1=ALU.mult), reads=[xt, ss, A], writes=[tmp])
        S.op('pool', lambda e: e.tensor_tensor(out=hb[:], in0=tmp[:], in1=SH[:], op=ALU.add),
             reads=[tmp, SH], writes=[hb])
        tp = b['tp'].next()
        S.op('pe', [(lambda e, k=k: e.transpose(out=tp[:, k, :], in_=hb[:, k * 128:(k + 1) * 128],
                                                identity=self.ident[:])) for k in range(8)],
             reads=[hb, self.ident], writes=[tp])
        S.op('act', lambda e: e.activation(out=hT[:, :, col:col + 128], in_=tp[:], func=AF.Copy),
             reads=[tp], writes=[hT])

    def store_hT(self, hT, HT, tok0, ncols):
        v = HT.rearrange("(k p) t -> p k t", p=128)
        self.S.dma('sp', v[:, :, 1 + tok0:1 + tok0 + ncols], hT[:, :, 0:ncols], reads=[hT], writes=["HT"])

    def norm_only(self, l, x_in, xc_in):
        S = self.S
        with self.phase() as ph:
            b = self.mk_norm_bufs(ph)
            xts = Rot([ph.sb("xt%d" % i, [128, D], F32) for i in range(3)])
            for which, src, n, HT in ((0, x_in, T, self.HTx), (1, xc_in, TC, self.HTc)):
                A = self.load_vec(ph, l, which, 0, "A")
                SH = self.load_vec(ph, l, which, 1, "SH")
                for tb in range(0, n, 512):
                    nb = min(512, n - tb)
                    hT = b['hT'].next()
                    for j in range(nb // 128):
                        xt = xts.next()
                        S.dma('sp', xt[:], src[tb + j * 128: tb + (j + 1) * 128, :], reads=["X"], writes=[xt])
                        self.norm_to_hT(xt, A, SH, b, hT, j * 128)
                    self.store_hT(hT, HT, tb, nb)

    def out_phase(self, l, kind, x_in, xc_in, x_out, xc_out, ctx_out):
        S = self.S
        with self.phase() as ph:
            if kind == 'mix':
                KC = 8
                wsrc = (self.wb["ev_w_out"][0] if l % 2 == 0 else self.wb["od_w_o"][0])[l // 2]
                gi, nxt = 2, (l, 3, 4)
            else:
                KC = 22
                wsrc = self.wb["ffn_w_down"][0][l]
                gi, nxt = 5, ((l + 1, 0, 1) if l + 1 < DEPTH else None)
            W = ph.sb("W", [128, KC, D], BF16)
            wv = wsrc.rearrange("(k p) n -> p k n", p=128)
            for k0 in range(0, KC, 4):
                k1 = min(KC, k0 + 4)
                S.dma('sp', W[:, k0:k1, :], wv[:, k0:k1, :], reads=["wb"], writes=[W])
            b = self.mk_norm_bufs(ph)
            xts = Rot([ph.sb("xt%d" % i, [128, D], F32) for i in range(2)])
            xns = Rot([ph.sb("xn%d" % i, [128, D], F32) for i in range(2)])
            ys = Rot([ph.ps("y%d" % i, [128, D]) for i in range(2)])
            if kind == 'mix':
                ains = Rot([ph.sb("ain%d" % i, [128, D], BF16) for i in range(2)])
                aTs = Rot([ph.sb("aT%d" % i, [128, 8, 128], BF16) for i in range(2)])
            else:
                aTs = Rot([ph.sb("aT%d" % i, [128, 22, 512], BF16) for i in range(2)])
            segs = [(0, x_in, x_out, T, 0, self.HTx)]
            if ctx_out:
                segs.append((1, xc_in, xc_out, TC, T, self.HTc))
            for which, xi, xo, n, off, HT in segs:
                G = self.load_vec(ph, l, which, gi, "G")
                if nxt is not None:
                    A = self.load_vec(ph, nxt[0], which, nxt[1], "A")
                    SH = self.load_vec(ph, nxt[0], which, nxt[2], "SH")
                for tb in range(0, n, 512):
                    nb = min(512, n - tb)
                    hT = b['hT'].next() if nxt is not None else None
                    if kind == 'ffn':
                        aT = aTs.next()
                        S.dma('sp', aT[:, :, 0:nb],
                              self.ACTT.rearrange("(k p) t -> p k t", p=128)[:, :, off + tb: off + tb + nb],
                              reads=["ACTT"], writes=[aT])
                    for j in range(nb // 128):
                        t0 = tb + j * 128
                        xt = xts.next()
                        S.dma('sp', xt[:], xi[t0:t0 + 128, :], reads=["X"], writes=[xt])
                        if kind == 'mix':
                            ain = ains.next()
                            S.dma('sp', ain[:], self.OMIX[off + t0: off + t0 + 128, :], reads=["OMIX"], writes=[ain])
                            tp = b['tp'].next()
                            S.op('pe', [(lambda e, k=k: e.transpose(out=tp[:, k, :], in_=ain[:, k * 128:(k + 1) * 128],
                                                                    identity=self.ident[:])) for k in range(8)],
                                 reads=[ain, self.ident], writes=[tp])
                            aT = aTs.next()
                            S.op('act', lambda e: e.activation(out=aT[:], in_=tp[:], func=AF.Copy),
                                 reads=[tp], writes=[aT])
                            c0 = 0
                        else:
                            c0 = j * 128
                        y = ys.next()
                        mms = []
                        for hf in range(2):
                            for k in range(KC):
                                mms.append(lambda e, k=k, hf=hf: e.matmul(
                                    y[:, hf * 512:(hf + 1) * 512], lhsT=aT[:, k, c0:c0 + 128],
                                    rhs=W[:, k, hf * 512:(hf + 1) * 512], start=(k == 0), stop=(k == KC - 1)))
                        S.op('pe', mms, reads=[aT, W], writes=[y])
                        ss = self.rstd_of(y[:], y, b, D)
                        tmp = b['tmp'].next()
                        xn = xns.next()
                        S.op('dve', lambda e: e.scalar_tensor_tensor(out=tmp[:], in0=y[:], scalar=ss[:, 1:2], in1=G[:],
                                                                     op0=ALU.mult, op1=ALU.mult),
                             reads=[y, ss, G], writes=[tmp])
                        S.op('pool', lambda e: e.tensor_tensor(out=xn[:], in0=tmp[:], in1=xt[:], op=ALU.add),
                             reads=[tmp, xt], writes=[xn])
                        S.dma('sp', xo[t0:t0 + 128, :], xn[:], reads=[xn], writes=["X"])
                        if nxt is not None:
                            self.norm_to_hT(xn, A, SH, b, hT, j * 128)
                    if nxt is not None:
                        self.store_hT(hT, HT, tb, nb)

    def conv3(self, eng, acc, ub, cw, cb, j, n=512):
        S = self.S
        S.op(eng, lambda e: e.tensor_scalar(out=acc[:, 0:n], in0=ub[:, 1:n + 1], scalar1=cw[:, 1, j:j + 1],
                                            scalar2=cb[:, j:j + 1], op0=ALU.mult, op1=ALU.add),
             reads=[ub, cw, cb], writes=[acc])
        S.op(eng, lambda e: e.scalar_tensor_tensor(out=acc[:, 0:n], in0=ub[:, 0:n], scalar=cw[:, 0, j:j + 1],
                                                   in1=acc[:, 0:n], op0=ALU.mult, op1=ALU.add),
             reads=[ub, cw, acc], writes=[acc])
        S.op(eng, lambda e: e.scalar_tensor_tensor(out=acc[:, 0:n], in0=ub[:, 2:n + 2], scalar=cw[:, 2, j:j + 1],
                                                   in1=acc[:, 0:n], op0=ALU.mult, op1=ALU.add),
             reads=[ub, cw, acc], writes=[acc])

    def mm_halo(self, pm, phl, W, hT, col_lo, col_n, n):
        mms = []
        for k in range(8):
            mms.append(lambda e, k=k: e.matmul(pm[:, 0:n], lhsT=W[:, k, col_lo:col_lo + col_n], rhs=hT[:, k, 1:n + 1],
                                               start=(k == 0), stop=(k == 7)))
        if phl is not None:
            for k in range(8):
                mms.append(lambda e, k=k: e.matmul(phl[:, 0:2], lhsT=W[:, k, col_lo:col_lo + col_n],
                                                   rhs=hT[:, k, 0:n + 2:n + 1], start=(k == 0), stop=(k == 7)))
        return mms

    def ffn1(self, l, ctx_out):
        S = self.S
        NJ = FH // 128
        with self.phase() as ph:
            W = ph.sb("Wup", [128, 8, 2 * FH], BF16)
            wv = self.wb["ffn_w_up"][0][l].rearrange("(k p) n -> p k n", p=128)
            for k in range(8):
                S.dma('sp', W[:, k, :], wv[:, k, :], reads=["wb"], writes=[W])
            cw = ph.sb("cw", [128, 3, 2 * NJ], F32)
            cb = ph.sb("cb", [128, 2 * NJ], F32)
            S.dma('sp', cw[:], self.ffn_conv_w[l].rearrange("a (j p) -> p a j", p=128), writes=[cw],
                  allow_slow_non_contiguous=True)
            S.dma('sp', cb[:], self.ffn_conv_b[l:l + 1, :].rearrange("o (j p) -> p (o j)", p=128), writes=[cb],
                  allow_slow_non_contiguous=True)
            hTs = Rot([ph.sb("hTi%d" % i, [128, 8, 514], BF16) for i in range(2)])
            pms = Rot([ph.ps("pm%d" % i, [128, 512]) for i in range(6)])
            hbank = ph.ps("hbank", [128, 512])
            phs = Rot([View(hbank[:, 8 * i:8 * i + 8], key=hbank) for i in range(8)])
            ubs = Rot([ph.sb("ub%d" % i, [128, 514], F32) for i in range(4)])
            accs = Rot([ph.sb("acc%d" % i, [128, 512], F32) for i in range(4)])
            sgs = Rot([ph.sb("sg%d" % i, [128, 512], F32) for i in range(2)])
            outs = Rot([ph.sb("ao%d" % i, [128, NJ, 512], BF16) for i in range(2)])
            segs = [(self.HTx, T, 0)]
            if ctx_out:
                segs.append((self.HTc, TC, T))
            for HT, n, off in segs:
                hv = HT.rearrange("(k p) t -> p k t", p=128)
                for tb in range(0, n, 512):
                    nb = min(512, n - tb)
                    hT = hTs.next()
                    S.dma('sp', hT[:, :, 0:nb + 2], hv[:, :, tb:tb + nb + 2], reads=["HT", "HTpad"], writes=[hT])
                    ao = outs.next()
                    for j in range(NJ):
                        accl = []
                        for half in range(2):
                            jj = half * NJ + j
                            pm = pms.next()
                            phl = phs.next()
                            S.op('pe', self.mm_halo(pm, phl, W, hT, jj * 128, 128, nb), reads=[W, hT], writes=[pm, phl])
                            ub = ubs.next()
                            S.op('act', lambda e: e.activation(out=ub[:, 1:nb + 1], in_=pm[:, 0:nb], func=AF.Copy),
                                 reads=[pm], writes=[ub])
                            S.op('act', lambda e: e.activation(out=ub[:, 0:nb + 2:nb + 1], in_=phl[:, 0:2], func=AF.Copy),
                                 reads=[phl, ub], writes=[ub])
                            acc = accs.next()
                            self.conv3('dve', acc, ub, cw, cb, jj, nb)
                            accl.append(acc)
                        sg = sgs.next()
                        S.op('act', lambda e: e.activation(out=sg[:, 0:nb], in_=accl[1][:, 0:nb], func=AF.Silu),
                             reads=[accl[1]], writes=[sg])
                        S.op('dve', lambda e: e.tensor_tensor(out=ao[:, j, 0:nb], in0=sg[:, 0:nb], in1=accl[0][:, 0:nb],
                                                              op=ALU.mult), reads=[sg, accl[0]], writes=[ao])
                    S.dma('sp', self.ACTT.rearrange("(k p) t -> p k t", p=128)[:, :, off + tb: off + tb + nb],
                          ao[:, :, 0:nb], reads=[ao], writes=["ACTT"])


    def even_mixer(self, l):
        ctx_out = (l < DEPTH - 1)
        self.ev_inproj(l)
        self.na_attn(l, ctx_out)
        self.mlstm_scan(l)
        self.mlstm_out(l, ctx_out)

    def ev_inproj(self, l):
        S = self.S
        e_ = l // 2
        with self.phase() as ph:
            W = ph.sb("Win", [128, 8, 3600], BF16)
            wv = self.wb["ev_w_in"][0][e_].rearrange("(k p) n -> p k n", p=128)
            for k in range(8):
                S.dma('sp', W[:, k, :], wv[:, k, :], reads=["wb"], writes=[W])
            cw = ph.sb("cw", [128, 3, 8], F32)
            cb = ph.sb("cb", [128, 8], F32)
            gb = ph.sb("gb", [128, 16], F32)
            S.dma('sp', cw[:], self.ev_conv_w[e_].rearrange("a (j p) -> p a j", p=128), writes=[cw], allow_slow_non_contiguous=True)
            S.dma('sp', cb[:], self.ev_conv_b[e_:e_ + 1, :].rearrange("o (j p) -> p (o j)", p=128), writes=[cb],
                  allow_slow_non_contiguous=True)
            S.dma('sp', gb[:], self.ev_gate_b[e_:e_ + 1, :].partition_broadcast(128), writes=[gb])
            hTs = Rot([ph.sb("hTi%d" % i, [128, 8, 514], BF16) for i in range(2)])
            pms = Rot([ph.ps("pm%d" % i, [128, 512]) for i in range(3)])
            hbank = ph.ps("hbank", [128, 512])
            phs = Rot([View(hbank[:, 8 * i:8 * i + 8], key=hbank) for i in range(8)])
            pts = Rot([ph.ps("pt%d" % i, [128, 512]) for i in range(3)])
            pg = ph.ps("pg", [128, 16])
            ubs = Rot([ph.sb("ub%d" % i, [128, 514], F32) for i in range(2)])
            accs = Rot([ph.sb("acc%d" % i, [128, 512], F32) for i in range(2)])
            sgs = Rot([ph.sb("sg%d" % i, [128, 512], F32) for i in range(2)])
            fms = Rot([ph.sb("fm%d" % i, [128, 16, 512], BF16) for i in range(2)])
            tms = Rot([ph.sb("tm%d" % i, [128, 4, 3, 512], BF16) for i in range(2)])
            gts = Rot([ph.sb("gt%d" % i, [128, 4, 16], F32) for i in range(2)])
            ges = Rot([ph.sb("ge%d" % i, [128, 2, 4], F32) for i in range(2)])
            qscale = 128 ** -0.5
            for HT, n, off in ((self.HTx, T, 0), (self.HTc, TC, T)):
                hv = HT.rearrange("(k p) t -> p k t", p=128)
                for tb in range(0, n, 512):
                    nb = min(512, n - tb)
                    hT = hTs.next()
                    S.dma('sp', hT[:, :, 0:nb + 2], hv[:, :, tb:tb + nb + 2], reads=["HT", "HTpad"], writes=[hT])
                    fm = fms.next()
                    for ch in range(8):
                        pm = pms.next()
                        S.op('pe', self.mm_halo(pm, None, W, hT, ch * 128, 128, nb), reads=[W, hT], writes=[pm])
                        S.op('act', lambda e: e.activation(out=fm[:, ch, 0:nb], in_=pm[:, 0:nb], func=AF.Copy), reads=[pm], writes=[fm])
                    for j in range(8):
                        pm = pms.next()
                        phl = phs.next()
                        S.op('pe', self.mm_halo(pm, phl, W, hT, 1536 + j * 128, 128, nb), reads=[W, hT], writes=[pm, phl])
                        ub = ubs.next()
                        S.op('act', lambda e: e.activation(out=ub[:, 1:nb + 1], in_=pm[:, 0:nb], func=AF.Copy), reads=[pm], writes=[ub])
                        S.op('act', lambda e: e.activation(out=ub[:, 0:nb + 2:nb + 1], in_=phl[:, 0:2], func=AF.Copy),
                             reads=[phl, ub], writes=[ub])
                        acc = accs.next()
                        self.conv3('dve', acc, ub, cw, cb, j, nb)
                        if j < 4:
                            sg = sgs.next()
                            S.op('act', lambda e: e.activation(out=sg[:, 0:nb], in_=acc[:, 0:nb], func=AF.Silu), reads=[acc], writes=[sg])
                            S.op('act', lambda e: e.activation(out=fm[:, 8 + j, 0:nb], in_=sg[:, 0:nb], func=AF.Copy, scale=qscale),
                                 reads=[sg], writes=[fm])
                        else:
                            S.op('act', lambda e: e.activation(out=fm[:, 8 + j, 0:nb], in_=acc[:, 0:nb], func=AF.Silu),
                                 reads=[acc], writes=[fm])
                    cs_ = slice(off + tb, off + tb + nb)
                    S.dma('sp', self.QTN.rearrange("(c p) t -> p c t", p=128)[:, :, cs_], fm[:, 0:4, 0:nb], reads=[fm], writes=["QTN"])
                    S.dma('sp', self.KTN.rearrange("(c p) t -> p c t", p=128)[:, :, cs_], fm[:, 4:8, 0:nb], reads=[fm], writes=["KTN"])
                    S.dma('sp', self.QKTM.rearrange("(c p) t -> p c t", p=128)[:, :, cs_], fm[:, 8:16, 0:nb], reads=[fm], writes=["QKTM"])
                    tm = tms.next()
                    gt = gts.next()
                    nj = nb // 128
                    for j in range(nj):
                        lhs = lambda k: hT[:, k, 1 + j * 128:1 + (j + 1) * 128]
                        for gi, c0 in enumerate((1024, 2560, 3072)):
                            pt = pts.next()
                            S.op('pe', [(lambda e, k=k: e.matmul(pt[:], lhsT=lhs(k), rhs=W[:, k, c0:c0 + 512], start=(k == 0), stop=(k == 7)))
                                        for k in range(8)], reads=[W, hT], writes=[pt])
                            if gi == 1:
                                S.op('dve', lambda e: e.tensor_copy(out=tm[:, j, gi, :], in_=pt[:]), reads=[pt], writes=[tm])
                            else:
                                S.op('act', lambda e: e.activation(out=tm[:, j, gi, :], in_=pt[:], func=AF.Copy), reads=[pt], writes=[tm])
                        S.op('pe', [(lambda e, k=k: e.matmul(pg[:], lhsT=lhs(k), rhs=W[:, k, 3584:3600], start=(k == 0), stop=(k == 7)))
                                    for k in range(8)], reads=[W, hT], writes=[pg])
                        S.op('dve', lambda e: e.tensor_tensor(out=gt[:, j, :], in0=pg[:], in1=gb[:], op=ALU.add), reads=[pg, gb], writes=[gt])
                        fv = gt[:, j, :].rearrange("p (a b) -> p a b", b=4)[:, 1:4:2, :]
                        ge = ges.next()
                        S.op('act', lambda e: e.activation(out=ge[:], in_=fv, func=AF.Exp, scale=-1.0), reads=[gt], writes=[ge])
                        S.op('dve', lambda e: e.tensor_scalar_add(out=ge[:], in0=ge[:], scalar1=1.0), reads=[ge], writes=[ge])
                        S.op('act', lambda e: e.activation(out=ge[:], in_=ge[:], func=AF.Ln), reads=[ge], writes=[ge])
                        S.op('dve', lambda e: e.tensor_scalar(out=fv, in0=ge[:], scalar1=-1.0, scalar2=None, op0=ALU.mult),
                             reads=[ge], writes=[gt])
                    rows = lambda Dd: Dd[off + tb: off + tb + nb, :].rearrange("(j p) f -> p j f", p=128)
                    S.dma('sp', rows(self.VN), tm[:, 0:nj, 0, :], reads=[tm], writes=["VN"])
                    S.dma('sp', rows(self.VML), tm[:, 0:nj, 1, :], reads=[tm], writes=["VML"])
                    S.dma('sp', rows(self.OML), tm[:, 0:nj, 2, :], reads=[tm], writes=["OML"])
                    S.dma('sp', rows(self.GML), gt[:, 0:nj, :], reads=[gt], writes=["GML"])

    def na_attn(self, l, ctx_out):
        S = self.S
        e_ = l // 2
        with self.phase() as ph:
            rp = ph.sb("rp", [120, 31], F32)
            Ep = ph.sb("Ep", [120, 128], F32)
            S.dma('sp', rp[:], self.ev_rpb[e_], writes=[rp])
            S.op('dve', lambda e: e.memset(Ep[:], 0.0), writes=[Ep])
            S.op('dve', lambda e: e.tensor_scalar(out=Ep[:, 0:16], in0=rp[:, 15:31], scalar1=8.0, scalar2=None, op0=ALU.mult),
                 reads=[rp, Ep], writes=[Ep])
            S.op('dve', lambda e: e.tensor_scalar(out=Ep[:, 113:128], in0=rp[:, 0:15], scalar1=8.0, scalar2=None, op0=ALU.mult),
                 reads=[rp, Ep], writes=[Ep])
            import os
            BTr = ph.sb("BTr", [64, 120, 64], F32)
            if os.environ.get("NA_SKIP") != "rb":
                S.dma('sp', self.RB.rearrange("a (r c) -> a r c", c=128), Ep[:].unsqueeze(1).broadcast_to([120, 64, 128]),
                      reads=[Ep], writes=["RB"])
                S.dma('sp', BTr[:], bass.AP(self.RB.tensor, 0, [[127, 64], [8192, 120], [1, 64]]), reads=["RB"], writes=[BTr])
            else:
                S.op('dve', lambda e: e.memset(BTr[:], 0.0), writes=[BTr])
            win = ph.sb("win", [64, 64], F32)
            S.dma('sp', win[:], self.k_win, writes=[win])
            BT = ph.sb("BT", [64, 120, 64], BF16)
            S.op('dve', lambda e: e.tensor_tensor(out=BT[:], in0=BTr[:], in1=win[:].unsqueeze(1).broadcast_to([64, 120, 64]), op=ALU.add),
                 reads=[BTr, win], writes=[BT])
            I64 = self.ident[0:64, 0:64]
            KTs = Rot([ph.sb("KTn%d" % i, [64, 2, TT], BF16) for i in range(2)])
            QTs = Rot([ph.sb("QTn%d" % i, [64, 2, TT], BF16) for i in range(2)])
            Vas = Rot([ph.sb("Va%d" % i, [128, 32, 2, 65], BF16) for i in range(2)])
            Vbs = Rot([ph.sb("Vb%d" % i, [128, 31, 2, 65], BF16) for i in range(2)])
            Vcs = Rot([ph.sb("Vc%d" % i, [128, 2, 2, 65], BF16) for i in range(2)])
            for v in Vas.tiles + Vbs.tiles + Vcs.tiles:
                S.op('dve', lambda e, v=v: e.memset(v[:, :, :, 64:65], 1.0), writes=[v])
            Sps = Rot([ph.ps("S%d" % i, [128, 6, 64]) for i in range(3)])
            Ops = Rot([ph.ps("O%d" % i, [64, 65]) for i in range(2)])
            Sc = ph.ps("Sc", [128, 2, 256])
            Ocs = [ph.ps("Oc%d" % i, [128, 65]) for i in range(2)]
            Ps = Rot([ph.sb("P%d" % i, [128, 6, 64], BF16) for i in range(3)])
            Pc = ph.sb("Pc", [128, 2, 256], BF16)
            rcs = Rot([ph.sb("rc%d" % i, [128, 1], F32) for i in range(4)])
            osts = Rot([ph.sb("ost%d" % i, [64, 8, 128], BF16) for i in range(2)])
            ostc = ph.sb("ostc", [128, 2, 128], BF16)
            ROWS = T // GRID
            import os
            NHP = int(os.environ.get('NA_NHP', '4'))
            for hp in range(NHP):
                KTt, QTt, Va, Vb, Vc = KTs.next(), QTs.next(), Vas.next(), Vbs.next(), Vcs.next()
                S.dma('sp', KTt[:], self.KTN[hp * 128:(hp + 1) * 128, :].rearrange("(h d) t -> d h t", d=64), reads=["KTN"], writes=[KTt])
                S.dma('sp', QTt[:], self.QTN[hp * 128:(hp + 1) * 128, :].rearrange("(h d) t -> d h t", d=64), reads=["QTN"], writes=[QTt])
                for hh in range(2):
                    c0 = hp * 128 + hh * 64
                    S.dma('sp', Va[:, :, hh, 0:64], self.VN[0:T, c0:c0 + 64].rearrange("(i p) f -> p i f", p=128), reads=["VN"], writes=[Va])
                    S.dma('sp', Vb[:, :, hh, 0:64], self.VN[64:64 + 31 * 128, c0:c0 + 64].rearrange("(i p) f -> p i f", p=128),
                          reads=["VN"], writes=[Vb])
                    S.dma('sp', Vc[:, :, hh, 0:64], self.VN[T:TT, c0:c0 + 64].rearrange("(i p) f -> p i f", p=128), reads=["VN"], writes=[Vc])
                items = [(r, hh) for r in range(ROWS) for hh in range(2)]
                spl = {}

                def emit_s(i):
                    r, hh = items[i]
                    h = hp * 2 + hh
                    rs = min(max(r - 4, 0), ROWS - 8)
                    sp_ = Sps.next()
                    mms = []
                    for m in range(4):
                        tok0 = (rs + 2 * m) * 64
                        dr0 = rs + 2 * m - r + 7
                        mms.append(lambda e, m=m, tok0=tok0: e.matmul(sp_[:, m, :], lhsT=KTt[:, hh, tok0:tok0 + 128],
                                                                      rhs=QTt[:, hh, r * 64:(r + 1) * 64], start=True, stop=False))
                        mms.append(lambda e, m=m, dr0=dr0: e.matmul(sp_[:, m, :], lhsT=BT[:, h * 15 + dr0:h * 15 + dr0 + 2, :],
                                                                    rhs=I64, start=False, stop=True))
                    for m in range(2):
                        mms.append(lambda e, m=m: e.matmul(sp_[:, 4 + m, :], lhsT=KTt[:, hh, T + m * 128:T + (m + 1) * 128],
                                                           rhs=QTt[:, hh, r * 64:(r + 1) * 64], start=True, stop=True))
                    S.op('pe', mms, reads=[KTt, QTt, BT, self.ident], writes=[sp_])
                    spl[i] = sp_
                emit_s(0)
                for i, (r, hh) in enumerate(items):
                    if i + 1 < len(items):
                        emit_s(i + 1)
                    rs = min(max(r - 4, 0), ROWS - 8)
                    if r % 8 == 0 and hh == 0:
                        ost = osts.next()
                    pr = slice(hh * 64, (hh + 1) * 64)
                    sp_ = spl.pop(i)
                    P = Ps.next()
                    S.op('act', lambda e: e.activation(out=P[:], in_=sp_[:], func=AF.Exp, scale=0.125), reads=[sp_], writes=[P])
                    O = Ops.next()
                    mms = []
                    for m in range(6):
                        if m >= 4:
                            vv = Vc[:, m - 4, hh, :]
                        elif rs % 2 == 0:
                            vv = Va[:, (rs + 2 * m) // 2, hh, :]
                        else:
                            vv = Vb[:, (rs + 2 * m - 1) // 2, hh, :]
                        mms.append(lambda e, m=m, vv=vv: e.matmul(O[:], lhsT=P[:, m, :], rhs=vv, start=(m == 0), stop=(m == 5)))
                    S.op('pe', mms, reads=[P, Va, Vb, Vc], writes=[O])
                    rc = rcs.next()
                    S.op('dve', lambda e: e.reciprocal(out=rc[0:64, :], in_=O[:, 64:65]), reads=[O], writes=[rc])
                    S.op('dve', lambda e: e.tensor_scalar(out=ost[:, r % 8, pr], in0=O[:, 0:64], scalar1=rc[0:64, 0:1], scalar2=None,
                                                          op0=ALU.mult), reads=[O, rc], writes=[ost])
                    if r % 8 == 7 and hh == 1:
                        r0 = r - 7
                        S.dma('sp', self.OMIX[r0 * 64:(r0 + 8) * 64, hp * 128:(hp + 1) * 128].rearrange("(rr p) f -> p rr f", p=64),
                              ost[:], reads=[ost], writes=["OMIX"])
                if ctx_out:
                    for hh in range(2):
                        pr = slice(hh * 64, (hh + 1) * 64)
                        S.op('pe', [(lambda e, kc=kc: e.matmul(Sc[:, kc, :], lhsT=KTt[:, hh, T + kc * 128:T + (kc + 1) * 128], rhs=QTt[:, hh, T:TT],
                                                               start=True, stop=True)) for kc in range(2)], reads=[KTt, QTt], writes=[Sc])
                        S.op('act', lambda e: e.activation(out=Pc[:], in_=Sc[:], func=AF.Exp, scale=0.125), reads=[Sc], writes=[Pc])
                        for qt in range(2):
                            O = Ocs[qt]
                            S.op('pe', [(lambda e, kc=kc: e.matmul(O[:], lhsT=Pc[:, kc, qt * 128:(qt + 1) * 128], rhs=Vc[:, kc, hh, :],
                                                                   start=(kc == 0), stop=(kc == 1))) for kc in range(2)],
                                 reads=[Pc, Vc], writes=[O])
                            rc = rcs.next()
                            S.op('dve', lambda e: e.reciprocal(out=rc[:], in_=O[:, 64:65]), reads=[O], writes=[rc])
                            S.op('dve', lambda e: e.tensor_scalar(out=ostc[:, qt, pr], in0=O[:, 0:64], scalar1=rc[:, 0:1], scalar2=None,
                                                                  op0=ALU.mult), reads=[O, rc], writes=[ostc])
                    S.dma('sp', self.OMIX[T:TT, hp * 128:(hp + 1) * 128].rearrange("(q p) f -> p q f", p=128), ostc[:],
                          reads=[ostc], writes=["OMIX"])

    def mlstm_scan(self, l):
        S = self.S
        with self.phase() as ph:
            tri = ph.sb("tri", [128, 4, 128], F32)
            S.dma('sp', tri[:], self.k_tri.rearrange("a p t -> p a t"), writes=[tri])
            NCH = TT // 128
            order = {0: [32, 33] + list(range(32)), 1: [33, 32] + list(range(31, -1, -1))}
            ST = [[ph.sb("ST%d_%d" % (d, h), [128, 129], F32) for h in range(4)] for d in range(2)]
            STb = [[ph.sb("STb%d_%d" % (d, h), [128, 129], BF16) for h in range(4)] for d in range(2)]
            for d in range(2):
                for h in range(4):
                    S.op('dve', lambda e, t=ST[d][h]: e.memset(t[:], 0.0), writes=[ST[d][h]])
                    S.op('dve', lambda e, t=STb[d][h]: e.memset(t[:], 0.0), writes=[STb[d][h]])
            QKs = Rot([ph.sb("QK%d" % i, [128, 8, 128], BF16) for i in range(4)])
            Vs = Rot([ph.sb("Vm%d" % i, [128, 4, 129], BF16) for i in range(4)])
            for v in Vs.tiles:
                S.op('dve', lambda e, v=v: e.memset(v[:, :, 128:129], 1.0), writes=[v])
            Gs = Rot([ph.sb("Gm%d" % i, [128, 16], F32) for i in range(4)])
            lfbs = Rot([ph.sb("lfb%d" % i, [128, 128], F32) for i in range(3)])
            css = Rot([ph.sb("cs%d" % i, [128, 4], F32) for i in range(6)])
            Dms = Rot([ph.sb("Dm%d" % i, [128, 128], F32) for i in range(3)])
            DTs = Rot([ph.sb("DT%d" % i, [128, 128], F32) for i in range(3)])
            abcs = Rot([ph.sb("abc%d" % i, [128, 128], F32) for i in range(3)])
            WTs = Rot([ph.sb("WT%d" % i, [128, 128], BF16) for i in range(3)])
            QaTs = Rot([ph.sb("QaT%d" % i, [128, 128], BF16) for i in range(3)])
            Kts = Rot([ph.sb("Kt%d" % i, [128, 128], BF16) for i in range(3)])
            Vws = Rot([ph.sb("Vw%d" % i, [128, 129], BF16) for i in range(3)])
            hsts = Rot([ph.sb("hst%d" % i, [128, 4, 128], F32) for i in range(4)])
            Bps = Rot([ph.ps("Bps%d" % i, [128, 129]) for i in range(2)])
            Sps = Rot([ph.ps("Sps%d" % i, [128, 128]) for i in range(2)])
            Hps = Rot([ph.ps("Hps%d" % i, [128, 129]) for i in range(2)])
            Tps = Rot([ph.ps("Tps%d" % i, [128, 128], BF16) for i in range(1)])
            KVps = Rot([ph.ps("KVps%d" % i, [128, 129]) for i in range(1)])
            qkv = self.QKTM.rearrange("(c p) t -> p c t", p=128)
            for step in range(NCH):
                for d in range(2):
                    ch = order[d][step]
                    cols = slice(ch * 128, (ch + 1) * 128)
                    QK, Vm, G = QKs.next(), Vs.next(), Gs.next()
                    S.dma('sp', QK[:], qkv[:, :, cols], reads=["QKTM"], writes=[QK])
                    S.dma('sp', Vm[:, :, 0:128], self.VML[cols, :].rearrange("p (h d) -> p h d", d=128), reads=["VML"], writes=[Vm])
                    S.dma('sp', G[:], self.GML[cols, :], reads=["GML"], writes=[G])
                    hst = hsts.next()
                    TRI = tri[:, d, :]
                    NEG = tri[:, 2 + d, :]
                    last = 127 if d == 0 else 0
                    for h in range(4):
                        icol = G[:, 8 * d + h:8 * d + h + 1]
                        lcol = G[:, 8 * d + 4 + h:8 * d + 4 + h + 1]
                        lfb = lfbs.next()
                        S.op('pool', lambda e: e.tensor_copy(out=lfb[:], in_=lcol.broadcast_to([128, 128])), reads=[G], writes=[lfb])
                        bp = Bps.next()
                        S.op('pe', [lambda e: e.matmul(bp[:, 0:128], lhsT=lfb[:], rhs=TRI, start=True, stop=True),
                                    lambda e: e.matmul(bp[:, 128:129], lhsT=TRI, rhs=lcol, start=True, stop=True)],
                             reads=[lfb, tri, G], writes=[bp])
                        cs = css.next()
                        S.op('dve', lambda e: e.tensor_tensor(out=cs[:, 0:1], in0=icol, in1=bp[:, 128:129], op=ALU.subtract),
                             reads=[G, bp], writes=[cs])
                        Dm = Dms.next()
                        S.op('dve', lambda e: e.tensor_tensor(out=Dm[:], in0=bp[:, 0:128], in1=NEG, op=ALU.add), reads=[bp, tri], writes=[Dm])
                        DT = DTs.next()
                        S.op('act', lambda e: e.activation(out=DT[:], in_=Dm[:], func=AF.Exp, bias=cs[:, 0:1]), reads=[Dm, cs], writes=[DT])
                        abc = abcs.next()
                        S.op('act', lambda e: e.activation(out=abc[:], in_=bp[:, 0:128], func=AF.Exp), reads=[bp], writes=[abc])
                        sp_ = Sps.next()
                        S.op('pe', lambda e: e.matmul(sp_[:], lhsT=QK[:, 4 + h, :], rhs=QK[:, h, :], start=True, stop=True),
                             reads=[QK], writes=[sp_])
                        WT = WTs.next()
                        S.op('dve', lambda e: e.tensor_tensor(out=WT[:], in0=sp_[:], in1=DT[:], op=ALU.mult), reads=[sp_, DT], writes=[WT])
                        QaT = QaTs.next()
                        S.op('pool', lambda e: e.tensor_tensor(out=QaT[:], in0=QK[:, h, :], in1=abc[:], op=ALU.mult),
                             reads=[QK, abc], writes=[QaT])
                        hp_ = Hps.next()
                        S.op('pe', [lambda e: e.matmul(hp_[:], lhsT=WT[:], rhs=Vm[:, h, :], start=True, stop=False),
                                    lambda e: e.matmul(hp_[:], lhsT=QaT[:], rhs=STb[d][h][:], start=False, stop=True)],
                             reads=[WT, Vm, QaT, STb[d][h]], writes=[hp_])
                        S.op('dve', lambda e: e.tensor_scalar(out=cs[:, 1:2], in0=hp_[:, 128:129], scalar1=-1.0, scalar2=None, op0=ALU.mult),
                             reads=[hp_, cs], writes=[cs])
                        S.op('dve', lambda e: e.tensor_tensor(out=cs[:, 1:2], in0=cs[:, 1:2], in1=hp_[:, 128:129], op=ALU.max),
                             reads=[hp_, cs], writes=[cs])
                        S.op('dve', lambda e: e.tensor_scalar_max(out=cs[:, 1:2], in0=cs[:, 1:2], scalar1=1.0), reads=[cs], writes=[cs])
                        S.op('dve', lambda e: e.reciprocal(out=cs[:, 2:3], in_=cs[:, 1:2]), reads=[cs], writes=[cs])
                        S.op('act', lambda e: e.activation(out=hst[:, h, :], in_=hp_[:, 0:128], func=AF.Copy, scale=cs[:, 2:3]),
                             reads=[hp_, cs], writes=[hst])
                        tp = Tps.next()
                        S.op('pe', lambda e: e.transpose(out=tp[:], in_=QK[:, 4 + h, :], identity=self.ident[:]),
                             reads=[QK, self.ident], writes=[tp])
                        Kt = Kts.next()
                        S.op('act', lambda e: e.activation(out=Kt[:], in_=tp[:], func=AF.Copy), reads=[tp], writes=[Kt])
                        Vw = Vws.next()
                        S.op('dve', lambda e: e.tensor_scalar(out=Vw[:], in0=Vm[:, h, :], scalar1=DT[:, last:last + 1], scalar2=None,
                                                              op0=ALU.mult), reads=[Vm, DT], writes=[Vw])
                        kv = KVps.next()
                        S.op('pe', lambda e: e.matmul(kv[:], lhsT=Kt[:], rhs=Vw[:], start=True, stop=True), reads=[Kt, Vw], writes=[kv])
                        st = ST[d][h]
                        S.op('dve', lambda e: e.scalar_tensor_tensor(out=st[:], in0=st[:], scalar=abc[:, last:last + 1], in1=kv[:],
                                                                     op0=ALU.mult, op1=ALU.add), reads=[st, abc, kv], writes=[st])
                        S.op('act', lambda e: e.activation(out=STb[d][h][:], in_=st[:], func=AF.Copy), reads=[st], writes=[STb[d][h]])
                    S.dma('sp', (self.HF if d == 0 else self.HB)[cols, :], hst[:].rearrange("p h d -> p (h d)"), reads=[hst],
                          writes=["HF" if d == 0 else "HB"])

    def mlstm_out(self, l, ctx_out):
        S = self.S
        e_ = l // 2
        with self.phase() as ph:
            g = ph.sb("mlg", [128, 512], F32)
            S.dma('sp', g[:], self.ev_ml_norm_g[e_:e_ + 1, :].partition_broadcast(128), writes=[g])
            hfs = Rot([ph.sb("hf%d" % i, [128, 512], F32) for i in range(2)])
            hbs = Rot([ph.sb("hb%d" % i, [128, 512], F32) for i in range(2)])
            obs = Rot([ph.sb("ob%d" % i, [128, 512], BF16) for i in range(2)])
            sgs = Rot([ph.sb("sg%d" % i, [128, 512], F32) for i in range(2)])
            t1s = Rot([ph.sb("t1%d" % i, [128, 512], F32) for i in range(2)])
            t2s = Rot([ph.sb("t2%d" % i, [128, 512], F32) for i in range(2)])
            sts = Rot([ph.sb("st%d" % i, [128, 3, 4], F32) for i in range(2)])
            outs = Rot([ph.sb("mo%d" % i, [128, 512], BF16) for i in range(2)])
            ntok = TT if ctx_out else T
            for t0 in range(0, ntok, 128):
                rows = slice(t0, t0 + 128)
                hf, hb, ob = hfs.next(), hbs.next(), obs.next()
                S.dma('sp', hf[:], self.HF[rows, :], reads=["HF"], writes=[hf])
                S.dma('sp', hb[:], self.HB[rows, :], reads=["HB"], writes=[hb])
                S.dma('sp', ob[:], self.OML[rows, :], reads=["OML"], writes=[ob])
                sg, t1, t2, st, mo = sgs.next(), t1s.next(), t2s.next(), sts.next(), outs.next()
                S.op('act', lambda e: e.activation(out=sg[:], in_=ob[:], func=AF.Sigmoid), reads=[ob], writes=[sg])
                S.op('pool', lambda e: e.tensor_tensor(out=t1[:], in0=hf[:], in1=hb[:], op=ALU.add), reads=[hf, hb], writes=[t1])
                S.op('dve', lambda e: e.tensor_tensor(out=t1[:], in0=t1[:], in1=sg[:], op=ALU.mult), reads=[t1, sg], writes=[t1])
                v1 = t1[:].rearrange("p (h d) -> p h d", d=128)
                v2 = t2[:].rearrange("p (h d) -> p h d", d=128)
                S.op('dve', lambda e: e.tensor_reduce(out=st[:, 0, :], in_=v1, axis=AX.X, op=ALU.add), reads=[t1], writes=[st])
                S.op('dve', lambda e: e.tensor_scalar(out=st[:, 0, :], in0=st[:, 0, :], scalar1=1.0 / 128, scalar2=None, op0=ALU.mult),
                     reads=[st], writes=[st])
                S.op('dve', lambda e: e.tensor_tensor(out=v1, in0=v1, in1=st[:, 0, :].unsqueeze(2).broadcast_to([128, 4, 128]),
                                                      op=ALU.subtract), reads=[t1, st], writes=[t1])
                S.op('pool', lambda e: e.tensor_tensor(out=t2[:], in0=t1[:], in1=t1[:], op=ALU.mult), reads=[t1], writes=[t2])
                S.op('dve', lambda e: e.tensor_reduce(out=st[:, 1, :], in_=v2, axis=AX.X, op=ALU.add), reads=[t2], writes=[st])
                S.op('dve', lambda e: e.tensor_scalar(out=st[:, 1, :], in0=st[:, 1, :], scalar1=1.0 / 128, scalar2=EPS,
                                                      op0=ALU.mult, op1=ALU.add), reads=[st], writes=[st])
                S.op('act', lambda e: e.activation(out=st[:, 1, :], in_=st[:, 1, :], func=AF.Sqrt), reads=[st], writes=[st])
                S.op('dve', lambda e: e.reciprocal(out=st[:, 2, :], in_=st[:, 1, :]), reads=[st], writes=[st])
                S.op('dve', lambda e: e.tensor_tensor(out=v2, in0=v1, in1=st[:, 2, :].unsqueeze(2).broadcast_to([128, 4, 128]),
                                                      op=ALU.mult), reads=[t1, st], writes=[t2])
                S.op('pool', lambda e: e.tensor_tensor(out=mo[:], in0=t2[:], in1=g[:], op=ALU.mult), reads=[t2, g], writes=[mo])
                S.dma('sp', self.OMIX[rows, 512:1024], mo[:], reads=[mo], writes=["OMIX"])


    def odd_mixer(self, l):
        self.mla_prep(l)
        self.mla_attn(l)

    def mla_prep(self, l):
        import os
        LVL = int(os.environ.get("PREP_LVL", "9"))
        S = self.S
        o = l // 2
        with self.phase() as ph:
            Wdq = ph.sb("Wdq", [128, 8, 256], BF16)
            Wdkv = ph.sb("Wdkv", [128, 8, 160], BF16)
            Wuq = ph.sb("Wuq", [128, 2, 1536], BF16)
            WukK = ph.sb("WukK", [128, 16, 64], BF16)
            WukV = ph.sb("WukV", [128, 16, 64], BF16)
            S.dma('sp', Wdq[:], self.wb["od_w_dq"][0][o].rearrange("(k p) n -> p k n", p=128), reads=["wb"], writes=[Wdq])
            S.dma('sp', Wdkv[:], self.wb["od_w_dkv"][0][o].rearrange("(k p) n -> p k n", p=128), reads=["wb"], writes=[Wdkv])
            S.dma('sp', Wuq[:], self.wb["od_w_uq"][0][o].rearrange("(k p) n -> p k n", p=128), reads=["wb"], writes=[Wuq])
            ukv = self.wb["od_w_ukv"][0][o].rearrange("p (h c) -> p h c", c=128)
            S.dma('sp', WukK[:], ukv[:, :, 0:64], reads=["wb"], writes=[WukK])
            S.dma('sp', WukV[:], ukv[:, :, 64:128], reads=["wb"], writes=[WukV])
            qg = ph.sb("qg", [128, 256], F32)
            kvg = ph.sb("kvg", [128, 128], F32)
            S.dma('sp', qg[:], self.od_q_norm_g[o:o + 1, :].partition_broadcast(128), writes=[qg])
            S.dma('sp', kvg[:], self.od_kv_norm_g[o:o + 1, :].partition_broadcast(128), writes=[kvg])
            b = {'junk': Rot([ph.sb("junk%d" % i, [128, 256], F32) for i in range(2)]),
                 'ss': Rot([ph.sb("ss%d" % i, [128, 2], F32) for i in range(6)])}
            hTs = Rot([ph.sb("hTi%d" % i, [128, 8, 512], BF16) for i in range(2)])
            pA = Rot([ph.ps("pA%d" % i, [128, 416]) for i in range(1)])
            pB = Rot([ph.ps("pB%d" % i, [128, 4, 128], BF16) for i in range(1)])
            pC = Rot([ph.ps("pC%d" % i, [128, 1536]) for i in range(1)])
            pD = Rot([ph.ps("pD%d" % i, [128, 12, 128], BF16) for i in range(1)])
            pE = Rot([ph.ps("pE%d" % i, [128, 512]) for i in range(1)])
            cqn = Rot([ph.sb("cqn%d" % i, [128, 256], BF16) for i in range(2)])
            ckvn = Rot([ph.sb("ckvn%d" % i, [128, 128], BF16) for i in range(2)])
            kpe = Rot([ph.sb("kpe%d" % i, [128, 128], BF16) for i in range(2)])
            for t_ in kpe.tiles:
                S.op('dve', lambda e, t_=t_: e.memset(t_[:], 0.0), writes=[t_])
            kpf = Rot([ph.sb("kpf%d" % i, [128, 32], F32) for i in range(2)])
            rt = Rot([ph.sb("rt%d" % i, [128, 16, 2, 8], F32) for i in range(4)])
            cqT = Rot([ph.sb("cqT%d" % i, [128, 2, 128], BF16) for i in range(2)])
            ckvT = Rot([ph.sb("ckvT%d" % i, [128, 512], BF16) for i in range(2)])
            kpeT = Rot([ph.sb("kpeT%d" % i, [128, 512], BF16) for i in range(2)])
            qtok = Rot([ph.sb("qtok%d" % i, [128, 16, 96], BF16) for i in range(2)])
            qfs = Rot([ph.sb("qf%d" % i, [128, 1536], F32) for i in range(2)])
            qTs = Rot([ph.sb("qTs%d" % i, [128, 12, 512], BF16) for i in range(2)])
            knT = Rot([ph.sb("knT%d" % i, [128, 512], BF16) for i in range(2)])
            vtk = Rot([ph.sb("vtk%d" % i, [128, 1024], BF16) for i in range(2)])
            for HT, n, off, rope in ((self.HTx, T, 0, True), (self.HTc, TC, T, False)):
                hv = HT.rearrange("(k p) t -> p k t", p=128)
                for tb in range(0, n, 512):
                    nb = min(512, n - tb)
                    hT = hTs.next()
                    S.dma('sp', hT[:, :, 0:nb], hv[:, :, 1 + tb:1 + tb + nb], reads=["HT"], writes=[hT])
                    ckT = ckvT.next()
                    kpT = kpeT.next()
                    qT = qTs.next()
                    for j in range(nb // 128):
                        tt = (tb // 128) + j
                        a = pA.next()
                        S.op('pe', [(lambda e, k=k: e.matmul(a[:, 0:256], lhsT=hT[:, k, j * 128:(j + 1) * 128], rhs=Wdq[:, k, :],
                                                             start=(k == 0), stop=(k == 7))) for k in range(8)] +
                             [(lambda e, k=k: e.matmul(a[:, 256:416], lhsT=hT[:, k, j * 128:(j + 1) * 128], rhs=Wdkv[:, k, :],
                                                       start=(k == 0), stop=(k == 7))) for k in range(8)],
                             reads=[hT, Wdq, Wdkv], writes=[a])
                        ssq = self.rstd_of(a[:, 0:256], a, b, 256)
                        cq = cqn.next()
                        S.op('dve', lambda e: e.scalar_tensor_tensor(out=cq[:], in0=a[:, 0:256], scalar=ssq[:, 1:2], in1=qg[:],
                                                                     op0=ALU.mult, op1=ALU.mult), reads=[a, ssq, qg], writes=[cq])
                        ssk = self.rstd_of(a[:, 256:384], a, b, 128)
                        ck = ckvn.next()
                        S.op('dve', lambda e: e.scalar_tensor_tensor(out=ck[:], in0=a[:, 256:384], scalar=ssk[:, 1:2], in1=kvg[:],
                                                                     op0=ALU.mult, op1=ALU.mult), reads=[a, ssk, kvg], writes=[ck])
                        kp = kpe.next()
                        if rope:
                            kf = kpf.next()
                            S.op('act', lambda e: e.activation(out=kf[:], in_=a[:, 384:416], func=AF.Copy), reads=[a], writes=[kf])
                            self.rope_tok(kf[:].rearrange("p (o a h j) -> p o a h j", o=1, a=2, h=2),
                                          kp[:, 0:32].rearrange("p (o a h j) -> p o a h j", o=1, a=2, h=2), 1, tt, rt, [kf], [kp])
                        else:
                            S.op('act', lambda e: e.activation(out=kp[:, 0:32], in_=a[:, 384:416], func=AF.Copy), reads=[a], writes=[kp])
                        if LVL < 1:
                            continue
                        tp = pB.next()
                        S.op('pe', [lambda e: e.transpose(out=tp[:, 0, :], in_=cq[:, 0:128], identity=self.ident[:]),
                                    lambda e: e.transpose(out=tp[:, 1, :], in_=cq[:, 128:256], identity=self.ident[:]),
                                    lambda e: e.transpose(out=tp[:, 2, :], in_=ck[:], identity=self.ident[:]),
                                    lambda e: e.transpose(out=tp[:, 3, :], in_=kp[:], identity=self.ident[:])],
                             reads=[cq, ck, kp, self.ident], writes=[tp])
                        cT = cqT.next()
                        S.op('act', lambda e: e.activation(out=cT[:], in_=tp[:, 0:2, :], func=AF.Copy), reads=[tp], writes=[cT])
                        S.op('act', lambda e: e.activation(out=ckT[:, j * 128:(j + 1) * 128], in_=tp[:, 2, :], func=AF.Copy),
                             reads=[tp], writes=[ckT])
                        S.op('act', lambda e: e.activation(out=kpT[:, j * 128:(j + 1) * 128], in_=tp[:, 3, :], func=AF.Copy),
                             reads=[tp], writes=[kpT])
                        if LVL < 2:
                            continue
                        cbig = pC.next()
                        S.op('pe', [(lambda e, k=k, nb_=nb_: e.matmul(cbig[:, nb_ * 512:(nb_ + 1) * 512], lhsT=cT[:, k, :],
                                                                      rhs=Wuq[:, k, nb_ * 512:(nb_ + 1) * 512],
                                                                      start=(k == 0), stop=(k == 1)))
                                    for nb_ in range(3) for k in range(2)], reads=[cT, Wuq], writes=[cbig])
                        qk = qtok.next()
                        qf = qfs.next()
                        for bk in range(3):
                            S.op('act', lambda e, bk=bk: e.activation(out=qf[:, bk * 512:(bk + 1) * 512], in_=cbig[:, bk * 512:(bk + 1) * 512],
                                                                      func=AF.Copy), reads=[cbig], writes=[qf])
                        qv = qf[:].rearrange("p (h c) -> p h c", c=96)
                        S.op('act', lambda e: e.activation(out=qk[:, :, 0:64], in_=qv[:, :, 0:64], func=AF.Copy),
                             reads=[qf], writes=[qk])
                        if rope:
                            self.rope_tok(qv[:, :, 64:96].rearrange("p h (a g j) -> p h a g j", a=2, g=2),
                                          qk[:, :, 64:96].rearrange("p h (a g j) -> p h a g j", a=2, g=2), 16, tt, rt,
                                          [qf], [qk])
                        else:
                            S.op('dve', lambda e: e.tensor_copy(out=qk[:, :, 64:96], in_=qv[:, :, 64:96]), reads=[qf], writes=[qk])
                        dT = pD.next()
                        qkf = qk[:].rearrange("p h c -> p (h c)")
                        S.op('pe', [(lambda e, c=c: e.transpose(out=dT[:, c, :], in_=qkf[:, c * 128:(c + 1) * 128], identity=self.ident[:]))
                                    for c in range(12)], reads=[qk, self.ident], writes=[dT])
                        S.op('act', lambda e: e.activation(out=qT[:, :, j * 128:(j + 1) * 128], in_=dT[:], func=AF.Copy),
                             reads=[dT], writes=[qT])
                        if LVL < 3:
                            continue
                        S.op('pe', [(lambda e, hf=hf: e.matmul(cbig[:, hf * 512:(hf + 1) * 512], lhsT=ckT[:, j * 128:(j + 1) * 128],
                                                               rhs=WukV[:, hf * 8:(hf + 1) * 8, :], start=True, stop=True))
                                    for hf in range(2)], reads=[ckT, WukV], writes=[cbig])
                        vt = vtk.next()
                        for bk in range(2):
                            S.op('dve', lambda e, bk=bk: e.tensor_copy(out=vt[:, bk * 512:(bk + 1) * 512], in_=cbig[:, bk * 512:(bk + 1) * 512]),
                                 reads=[cbig], writes=[vt])
                        S.dma('sp', self.VM[off + tb + j * 128: off + tb + (j + 1) * 128, :], vt[:], reads=[vt], writes=["VM"])
                    if LVL < 4:
                        continue
                    S.dma('sp', self.QT.rearrange("h f t -> (h f) t").rearrange("(c p) t -> p c t", p=128)[:, :, off + tb: off + tb + nb],
                          qT[:, :, 0:nb], reads=[qT], writes=["QT"])
                    if LVL < 5:
                        continue
                    for hp in range(8):
                        pe_ = pE.next()
                        S.op('pe', lambda e: e.matmul(pe_[:, 0:nb], lhsT=WukK[:, 2 * hp:2 * hp + 2, :], rhs=ckT[:, 0:nb],
                                                      start=True, stop=True), reads=[WukK, ckT], writes=[pe_])
                        kn = knT.next()
                        S.op('act', lambda e: e.activation(out=kn[:, 0:nb], in_=pe_[:, 0:nb], func=AF.Copy), reads=[pe_], writes=[kn])
                        for u in range(2):
                            S.dma('sp', self.KT[2 * hp + u, 0:64, off + tb: off + tb + nb], kn[u * 64:(u + 1) * 64, 0:nb],
                                  reads=[kn], writes=["KT"])
                    for h in range(16):
                        S.dma('sp', self.KT[h, 64:96, off + tb: off + tb + nb], kpT[0:32, 0:nb], reads=[kpT], writes=["KT"])

    def rope_tok(self, src, dst, nh, tt, rt, rk, wk):
        S = self.S
        cos = self.COS[:, tt, :].rearrange("p (a j) -> p a j", a=2).unsqueeze(1).broadcast_to([128, nh, 2, 8])
        sin = self.SIN[:, tt, :].rearrange("p (a j) -> p a j", a=2).unsqueeze(1).broadcast_to([128, nh, 2, 8])
        x1 = src[:, :, :, 0, :]
        x2 = src[:, :, :, 1, :]
        t = [rt.next() for _ in range(4)]
        tv = [q[:, 0:nh, :, :] for q in t]
        S.op('dve', lambda e: e.tensor_tensor(out=tv[0], in0=x1, in1=cos, op=ALU.mult), reads=rk + [self.COS], writes=[t[0]])
        S.op('dve', lambda e: e.tensor_tensor(out=tv[1], in0=x2, in1=sin, op=ALU.mult), reads=rk + [self.SIN], writes=[t[1]])
        S.op('dve', lambda e: e.tensor_tensor(out=tv[2], in0=x1, in1=sin, op=ALU.mult), reads=rk + [self.SIN], writes=[t[2]])
        S.op('dve', lambda e: e.tensor_tensor(out=tv[3], in0=x2, in1=cos, op=ALU.mult), reads=rk + [self.COS], writes=[t[3]])
        S.op('dve', lambda e: e.tensor_tensor(out=dst[:, :, :, 0, :], in0=tv[0], in1=tv[1], op=ALU.subtract),
             reads=[t[0], t[1]], writes=wk)
        S.op('dve', lambda e: e.tensor_tensor(out=dst[:, :, :, 1, :], in0=tv[2], in1=tv[3], op=ALU.add),
             reads=[t[2], t[3]], writes=wk)

    def mla_attn(self, l):
        S = self.S
        NKC = TT // 128
        scale = 96 ** -0.5
        with self.phase() as ph:
            KTs = Rot([ph.sb("KTs%d" % i, [128, 4, TT], BF16) for i in range(2)])
            for t_ in KTs.tiles:
                S.op('dve', lambda e, t_=t_: e.memset(t_[:], 0.0), writes=[t_])
            Vs = Rot([ph.sb("Vs%d" % i, [128, NKC, 4, 65], BF16) for i in range(2)])
            for v in Vs.tiles:
                S.op('dve', lambda e, v=v: e.memset(v[:, :, :, 64:65], 1.0), writes=[v])
            QTs = Rot([ph.sb("QTb%d" % i, [128, 4, 512], BF16) for i in range(2)])
            for t_ in QTs.tiles:
                S.op('dve', lambda e, t_=t_: e.memset(t_[:], 0.0), writes=[t_])
            Ps = Rot([ph.sb("P%d" % i, [128, 512], BF16) for i in range(4)])
            Sps = Rot([ph.ps("S%d" % i, [128, 512]) for i in range(4)])
            Obk = [ph.ps("O%d" % i, [128, 65]) for i in range(4)]
            rcs = Rot([ph.sb("rc%d" % i, [128, 4], F32) for i in range(4)])
            Ost = Rot([ph.sb("Ost%d" % i, [128, 4, 4, 64], BF16) for i in range(2)])
            ctx_out = (l < DEPTH - 1)
            qblocks = [(qb * 512, 512, list(range(NKC))) for qb in range(T // 512)]
            if ctx_out:
                qblocks.append((T, TC, [NKC - 2, NKC - 1]))
            for hg in range(4):
                KTt = KTs.next()
                Vt = Vs.next()
                S.dma('sp', KTt[0:96, :, :], self.KT.rearrange("h f t -> f h t")[:, hg * 4:(hg + 1) * 4, :], reads=["KT"], writes=[KTt])
                vsrc = self.VM.rearrange("(kc p) (h d) -> p kc h d", p=128, d=64)
                for h in range(4):
                    S.dma('sp', Vt[:, :, h, 0:64], vsrc[:, :, hg * 4 + h, :], reads=["VM"], writes=[Vt])
                for q0, nq, kcs in qblocks:
                    Qt = QTs.next()
                    S.dma('sp', Qt[0:96, :, 0:nq], self.QT.rearrange("h f t -> f h t")[:, hg * 4:(hg + 1) * 4, q0:q0 + nq],
                          reads=["QT"], writes=[Qt])
                    ost = Ost.next()
                    nqt = nq // 128
                    items = [(h, ci, kc) for h in range(4) for ci, kc in enumerate(kcs)]
                    spl = {}

                    def emit_s(i):
                        h, ci, kc = items[i]
                        sp_ = Sps.next()
                        S.op('pe', lambda e: e.matmul(sp_[:, 0:nq], lhsT=KTt[:, h, kc * 128:(kc + 1) * 128], rhs=Qt[:, h, 0:nq],
                                                      start=True, stop=True), reads=[KTt, Qt], writes=[sp_])
                        spl[i] = sp_
                    emit_s(0)
                    for i, (h, ci, kc) in enumerate(items):
                        if i + 1 < len(items):
                            emit_s(i + 1)
                        sp_ = spl.pop(i)
                        P = Ps.next()
                        S.op('act', lambda e: e.activation(out=P[:, 0:nq], in_=sp_[:, 0:nq], func=AF.Exp, scale=scale),
                             reads=[sp_], writes=[P])
                        S.op('pe', [(lambda e, qt=qt: e.matmul(Obk[qt][:], lhsT=P[:, qt * 128:(qt + 1) * 128], rhs=Vt[:, kc, h, :],
                                                               start=(ci == 0), stop=(ci == len(kcs) - 1)))
                                    for qt in range(nqt)], reads=[P, Vt], writes=Obk[0:nqt])
                        if ci == len(kcs) - 1:
                            rc = rcs.next()
                            for qt in range(nqt):
                                O = Obk[qt]
                                S.op('dve', lambda e: e.reciprocal(out=rc[:, qt:qt + 1], in_=O[:, 64:65]), reads=[O], writes=[rc])
                                S.op('dve', lambda e: e.tensor_scalar(out=ost[:, qt, h, :], in0=O[:, 0:64], scalar1=rc[:, qt:qt + 1],
                                                                      scalar2=None, op0=ALU.mult), reads=[O, rc], writes=[ost])
                    S.dma('sp', self.OMIX[q0:q0 + nq, hg * 256:(hg + 1) * 256].rearrange("(qt p) f -> p qt f", p=128),
                          ost[:, 0:nqt, :, :].rearrange("p q h d -> p q (h d)"), reads=[ost], writes=["OMIX"])


def host_consts():
    ident = np.eye(128, dtype=np.float32)
    U = np.triu(np.ones((128, 128), np.float32))
    L = U.T.copy()
    tri = np.stack([U, L, (U - 1.0) * 1e4, (L - 1.0) * 1e4]).astype(np.float32)
    pos = np.arange(T)
    row = (pos // GRID).astype(np.float32)
    col = (pos % GRID).astype(np.float32)
    kpos = np.concatenate([np.repeat(row[:, None], 8, 1), np.repeat(col[:, None], 8, 1)], axis=1).astype(np.float32)
    inv = (10000.0 ** (-(np.arange(8, dtype=np.float32)) / 8.0)).astype(np.float32)
    kinv = np.concatenate([inv, inv])[None, :].astype(np.float32)
    qc = np.arange(64)
    cs = np.clip(qc - 8, 0, 48)
    win = ((qc[None, :] >= cs[:, None]) & (qc[None, :] < cs[:, None] + 16))
    kwin = np.where(win, 0.0, -30000.0).astype(np.float32)
    return dict(k_ident=ident, k_tri=tri, k_pos=kpos, k_inv=kinv, k_win=kwin)


def make_in_maps(inputs, cores):
    hc = host_consts()
    f = lambda a: np.ascontiguousarray(np.asarray(a, dtype=np.float32))
    shared = {}
    for name in ("ada_w", "ffn_w_up", "ffn_conv_w", "ffn_conv_b", "ffn_w_down", "ev_w_in", "ev_gate_b",
                 "ev_conv_w", "ev_conv_b", "ev_ml_norm_g", "ev_w_out", "od_w_dq", "od_q_norm_g", "od_w_uq",
                 "od_w_dkv", "od_kv_norm_g", "od_w_ukv", "od_w_o", "ada_b"):
        shared[name] = f(inputs[name])
    shared["norm_g"] = f(inputs["norm_g"]).reshape(DEPTH, 4 * D)
    shared["ev_rpb"] = f(inputs["ev_rpb"]).reshape(2, 120, 31)
    shared["c_ctx"] = f(inputs["c_ctx"]).reshape(1, D)
    shared.update(hc)
    maps = []
    for b in cores:
        m = dict(shared)
        m["x"] = f(inputs["x"][b])
        m["c"] = f(inputs["c"][b]).reshape(1, D)
        m["ctx"] = f(inputs["ctx"][b])
        maps.append(m)
    return maps


def kernel(**inputs):
    kb = K()
    nc = kb.build()
    maps = make_in_maps(inputs, list(range(8)))
    res = run_bass_kernel_spmd(nc, maps, core_ids=list(range(8)))
    return np.stack([np.asarray(r["out"], dtype=np.float32) for r in res.results], axis=0)
```
